# Optimizing a Trainium2 kernel written in Bass

```python
import math
import jax
import jax.numpy as jnp
from jax import lax
import numpy as np

D_MODEL = 4096
BATCH = 4
SEQ = 2048
DEPTH = 2

GRID_W = 64
CTX_LEN = 256
HEAD_DIM = 128
MIX_WIDTH = D_MODEL
N_HEAD_SLOTS = MIX_WIDTH // HEAD_DIM
A_HEADS = N_HEAD_SLOTS // 4
B_HEADS = (N_HEAD_SLOTS - A_HEADS) // 2
C_HEADS = N_HEAD_SLOTS - A_HEADS - B_HEADS
A_WIDTH = A_HEADS * HEAD_DIM
CHUNK = 128
Q_LORA = 1536
KV_LORA = 512
QK_NOPE = 128
QK_ROPE = 64
V_DIM = HEAD_DIM
B_QK = QK_NOPE + QK_ROPE
C_KV_HEADS = C_HEADS // 3
C_GROUP = C_HEADS // C_KV_HEADS
WINDOW = 128
BLOCK = 128
PEER_HEADS = 8
N_KEYS = 128
N_EXPERTS = N_KEYS * N_KEYS
PEER_TOPK = 16
PEER_QDIM = 256
PEER_HALF = PEER_QDIM // 2
PEER_BLOCK = 64
ROPE_BASE = 10000.0
EPS = 1e-6
NEG = -1e30
N_MOD = 6
P_A = 2 * A_WIDTH
P_B = Q_LORA + KV_LORA + QK_ROPE
P_C = (C_HEADS + 2 * C_KV_HEADS) * HEAD_DIM
P_IN = P_A + P_B + P_C

kernel_name = 'hybrid_prefix_dit_block'


def rms_norm(x, gain):
    xf = x.astype(jnp.float32)
    y = xf * lax.rsqrt(jnp.mean(xf * xf, axis=-1, keepdims=True) + EPS)
    return (y * gain.astype(jnp.float32)).astype(x.dtype)


def modulate(h, shift, scale):
    return h * (1 + scale) + shift


def axial_rope_tables(row, col, rot_dim):
    n_freq = rot_dim // 4
    inv = ROPE_BASE ** (-jnp.arange(n_freq, dtype=jnp.float32) / n_freq)
    ar = row[:, None] * inv
    ac = col[:, None] * inv
    ang = jnp.concatenate([ar, ar, ac, ac], axis=-1)
    return jnp.cos(ang), jnp.sin(ang)


def apply_rope(x, cos, sin):
    xf = x.astype(jnp.float32)
    xs = xf.reshape(xf.shape[:-1] + (2, 2, -1))
    rot = jnp.concatenate([-xs[..., 1:, :], xs[..., :1, :]], axis=-2).reshape(xf.shape)
    return (xf * cos[:, None, :] + rot * sin[:, None, :]).astype(x.dtype)


def split_groups(p):
    return p[..., :P_A], p[..., P_A:P_A + P_B], p[..., P_A + P_B:]


def chunk_gmlp(p, v_gain, w_s, b_s):
    b, l, _ = p.shape
    z = jax.nn.gelu(p)
    u, v = z[..., :A_WIDTH], z[..., A_WIDTH:]
    v = rms_norm(v.reshape(b, l, A_HEADS, HEAD_DIM), v_gain.reshape(A_HEADS, HEAD_DIM))
    v = v.reshape(b, l // CHUNK, CHUNK, A_WIDTH)
    s = jnp.einsum('ts,bnsc->bntc', w_s, v) + b_s[:, None]
    return u * s.reshape(b, l, A_WIDTH)


def softmax_attend(q, k, v):
    s = jnp.einsum('bhqd,bhkd->bhqk', q, k).astype(jnp.float32) * (q.shape[-1] ** -0.5)
    p = jax.nn.softmax(s, axis=-1).astype(v.dtype)
    return jnp.einsum('bhqk,bhkd->bhqd', p, v)


def attend_blocked(q, k, v):
    b, h, l, d = q.shape
    qb = jnp.moveaxis(q.reshape(b, h, l // BLOCK, BLOCK, d), 2, 0)
    ob = lax.map(lambda qi: softmax_attend(qi, k, v), qb)
    return jnp.moveaxis(ob, 0, 2).reshape(b, h, l, -1)


def heads_to_tokens(o):
    b, h, l, d = o.shape
    return o.transpose(0, 2, 1, 3).reshape(b, l, h * d)


def sink_softmax(s, sink):
    sk = jnp.broadcast_to(sink, s.shape[:-1] + (1,))
    return jax.nn.softmax(jnp.concatenate([s, sk], axis=-1), axis=-1)[..., :-1]


def mla_qkv(p, q_gain, kv_gain, w_uq, w_ukv, qn_gain, kn_gain, rope):
    b, l, _ = p.shape
    c_q = p[..., :Q_LORA]
    c_kv = p[..., Q_LORA:Q_LORA + KV_LORA]
    k_r = p[..., Q_LORA + KV_LORA:]
    q = (rms_norm(c_q, q_gain) @ w_uq).reshape(b, l, B_HEADS, B_QK)
    kv = (rms_norm(c_kv, kv_gain) @ w_ukv).reshape(b, l, B_HEADS, QK_NOPE + V_DIM)
    k = jnp.concatenate([kv[..., :QK_NOPE],
                         jnp.broadcast_to(k_r[:, :, None, :], (b, l, B_HEADS, QK_ROPE))], axis=-1)
    v = kv[..., QK_NOPE:]
    q = rms_norm(q, qn_gain)
    k = rms_norm(k, kn_gain)
    if rope is not None:
        cos, sin = rope
        q = jnp.concatenate([q[..., :QK_NOPE], apply_rope(q[..., QK_NOPE:], cos, sin)], axis=-1)
        k = jnp.concatenate([k[..., :QK_NOPE], apply_rope(k[..., QK_NOPE:], cos, sin)], axis=-1)
    return q.transpose(0, 2, 1, 3), k.transpose(0, 2, 1, 3), v.transpose(0, 2, 1, 3)


def gqa_qkv(p, qn_gain, kn_gain, rope):
    b, l, _ = p.shape
    nq = C_HEADS * HEAD_DIM
    nk = C_KV_HEADS * HEAD_DIM
    q = rms_norm(p[..., :nq].reshape(b, l, C_HEADS, HEAD_DIM), qn_gain)
    k = rms_norm(p[..., nq:nq + nk].reshape(b, l, C_KV_HEADS, HEAD_DIM), kn_gain)
    v = p[..., nq + nk:].reshape(b, l, C_KV_HEADS, HEAD_DIM)
    if rope is not None:
        cos, sin = rope
        q = apply_rope(q, cos, sin)
        k = apply_rope(k, cos, sin)
    q = q.reshape(b, l, C_KV_HEADS, C_GROUP, HEAD_DIM).transpose(0, 2, 3, 1, 4)
    return q, k.transpose(0, 2, 1, 3), v.transpose(0, 2, 1, 3)


def window_attention_with_ctx(q, k, v, k_ctx, v_ctx, sink):
    b, hk, g, l, d = q.shape
    nb = l // BLOCK
    scale = d ** -0.5
    qb = q.reshape(b, hk, g, nb, BLOCK, d)

    def band(t):
        tb = t.reshape(b, hk, nb, BLOCK, -1)
        tp = jnp.pad(tb, ((0, 0), (0, 0), (1, 1), (0, 0), (0, 0)))
        return jnp.concatenate([tp[:, :, :-2], tp[:, :, 1:-1], tp[:, :, 2:]], axis=3)

    kb, vb = band(k), band(v)
    s_band = jnp.einsum('bkgnqd,bknjd->bkgnqj', qb, kb).astype(jnp.float32) * scale
    s_ctx = jnp.einsum('bkgnqd,bkjd->bkgnqj', qb, k_ctx).astype(jnp.float32) * scale
    blk = jnp.arange(nb)[:, None, None]
    qpos = blk * BLOCK + jnp.arange(BLOCK)[None, :, None]
    kpos = (blk - 1) * BLOCK + jnp.arange(3 * BLOCK)[None, None, :]
    valid = (jnp.abs(qpos - kpos) <= WINDOW) & (kpos >= 0) & (kpos < l)
    s_band = jnp.where(valid, s_band, NEG)
    sink_b = sink.astype(jnp.float32).reshape(1, hk, g, 1, 1, 1)
    p = sink_softmax(jnp.concatenate([s_ctx, s_band], axis=-1), sink_b)
    n_ctx = k_ctx.shape[2]
    o = (jnp.einsum('bkgnqj,bkjd->bkgnqd', p[..., :n_ctx].astype(v.dtype), v_ctx)
         + jnp.einsum('bkgnqj,bknjd->bkgnqd', p[..., n_ctx:].astype(v.dtype), vb))
    return o.reshape(b, hk, g, l, d)


def context_sink_attention(q, k, v, sink):
    hk, g = q.shape[1], q.shape[2]
    s = jnp.einsum('bkgqd,bkjd->bkgqj', q, k).astype(jnp.float32) * (q.shape[-1] ** -0.5)
    p = sink_softmax(s, sink.astype(jnp.float32).reshape(1, hk, g, 1, 1))
    return jnp.einsum('bkgqj,bkjd->bkgqd', p.astype(v.dtype), v)


def gqa_out(o):
    b, hk, g, l, d = o.shape
    return o.transpose(0, 3, 1, 2, 4).reshape(b, l, hk * g * d)


def peer_ffn(h, w_q, subkeys, u_tab, v_tab):
    b, l, dm = h.shape
    hb = h.reshape(-1, PEER_BLOCK, dm)

    def one(t):
        q = (t @ w_q).reshape(PEER_BLOCK, PEER_HEADS, 2, PEER_HALF)
        s = jnp.einsum('thpd,hpnd->thpn', q, subkeys).astype(jnp.float32)
        top_s, top_i = lax.top_k(s, PEER_TOPK)
        cand_s = (top_s[:, :, 0, :, None] + top_s[:, :, 1, None, :]).reshape(PEER_BLOCK, PEER_HEADS, -1)
        cand_i = (top_i[:, :, 0, :, None] * N_KEYS + top_i[:, :, 1, None, :]).reshape(PEER_BLOCK, PEER_HEADS, -1)
        best_s, best_j = lax.top_k(cand_s, PEER_TOPK)
        idx = jnp.take_along_axis(cand_i, best_j, axis=-1)
        gate = jax.nn.softmax(best_s, axis=-1)
        u = jnp.take(u_tab, idx, axis=0)
        act = jax.nn.gelu(jnp.einsum('td,thkd->thk', t, u).astype(jnp.float32))
        w = (gate * act).astype(t.dtype)
        v = jnp.take(v_tab, idx, axis=0)
        return jnp.einsum('thk,thkd->td', w, v)

    return lax.map(one, hb).reshape(b, l, dm)


def setup_inputs(seed: int = 0) -> dict:
    key = jax.random.key(seed)
    ks = jax.random.split(key, 32)
    f32 = jnp.float32
    L, D = DEPTH, D_MODEL

    def nrm(k, shape, scale):
        return jax.random.normal(k, shape, f32) * scale

    def gain(k, shape):
        return 1.0 + 0.02 * jax.random.normal(k, shape, f32)

    return {
        'x': nrm(ks[0], (BATCH, SEQ, D), 1.0),
        'c': nrm(ks[1], (BATCH, D), 1.0),
        'ctx': nrm(ks[2], (BATCH, CTX_LEN, D), 1.0),
        'c_ctx': nrm(ks[3], (D,), 1.0),
        'w_mod': nrm(ks[4], (L, D, N_MOD * D), 0.5 * D ** -0.5),
        'b_mod': nrm(ks[5], (L, N_MOD * D), 0.02),
        'norm1_gain': gain(ks[6], (L, D)),
        'norm2_gain': gain(ks[7], (L, D)),
        'w_in': nrm(ks[8], (L, D, P_IN), D ** -0.5),
        'a_v_gain': gain(ks[9], (L, A_WIDTH)),
        'a_w_s': nrm(ks[10], (L, CHUNK, CHUNK), CHUNK ** -0.5),
        'a_b_s': gain(ks[11], (L, CHUNK)),
        'b_q_gain': gain(ks[12], (L, Q_LORA)),
        'b_kv_gain': gain(ks[13], (L, KV_LORA)),
        'b_w_uq': nrm(ks[14], (L, Q_LORA, B_HEADS * B_QK), Q_LORA ** -0.5),
        'b_w_ukv': nrm(ks[15], (L, KV_LORA, B_HEADS * (QK_NOPE + V_DIM)), KV_LORA ** -0.5),
        'b_qn_gain': gain(ks[16], (L, B_QK)),
        'b_kn_gain': gain(ks[17], (L, B_QK)),
        'c_qn_gain': gain(ks[18], (L, HEAD_DIM)),
        'c_kn_gain': gain(ks[19], (L, HEAD_DIM)),
        'c_sink': nrm(ks[20], (L, C_HEADS), 0.5),
        'w_out': nrm(ks[21], (L, MIX_WIDTH, D), MIX_WIDTH ** -0.5),
        'peer_w_q': nrm(ks[22], (L, D, PEER_HEADS * PEER_QDIM), D ** -0.5),
        'peer_subkeys': nrm(ks[23], (L, PEER_HEADS, 2, N_KEYS, PEER_HALF), PEER_HALF ** -0.5),
        'peer_u': nrm(ks[24], (L, N_EXPERTS, D), D ** -0.5),
        'peer_v': nrm(ks[25], (L, N_EXPERTS, D), PEER_HEADS ** -0.5),
    }


def reference(x, c, ctx, c_ctx, w_mod, b_mod, norm1_gain, norm2_gain, w_in, a_v_gain, a_w_s, a_b_s,
              b_q_gain, b_kv_gain, b_w_uq, b_w_ukv, b_qn_gain, b_kn_gain, c_qn_gain, c_kn_gain, c_sink,
              w_out, peer_w_q, peer_subkeys, peer_u, peer_v):
    s_len = x.shape[1]
    rows = s_len // GRID_W
    row = jnp.repeat(jnp.arange(rows, dtype=jnp.float32), GRID_W)
    col = jnp.tile(jnp.arange(GRID_W, dtype=jnp.float32), rows)
    rope_b = axial_rope_tables(row, col, QK_ROPE)
    rope_c = axial_rope_tables(row, col, HEAD_DIM)
    silu_c = jax.nn.silu(c)
    silu_cc = jax.nn.silu(c_ctx)

    for layer in range(DEPTH):
        need_ctx = layer < DEPTH - 1
        m_lat = jnp.split((silu_c @ w_mod[layer] + b_mod[layer])[:, None, :], N_MOD, axis=-1)
        m_ctx = jnp.split(silu_cc @ w_mod[layer] + b_mod[layer], N_MOD, axis=-1)

        h_lat = modulate(rms_norm(x, norm1_gain[layer]), m_lat[0], m_lat[1])
        h_ctx = modulate(rms_norm(ctx, norm1_gain[layer]), m_ctx[0], m_ctx[1])
        pa_lat, pb_lat, pc_lat = split_groups(h_lat @ w_in[layer])
        pa_ctx, pb_ctx, pc_ctx = split_groups(h_ctx @ w_in[layer])

        oa_lat = chunk_gmlp(pa_lat, a_v_gain[layer], a_w_s[layer], a_b_s[layer])

        qb_l, kb_l, vb_l = mla_qkv(pb_lat, b_q_gain[layer], b_kv_gain[layer], b_w_uq[layer], b_w_ukv[layer],
                                   b_qn_gain[layer], b_kn_gain[layer], rope_b)
        qb_c, kb_c, vb_c = mla_qkv(pb_ctx, b_q_gain[layer], b_kv_gain[layer], b_w_uq[layer], b_w_ukv[layer],
                                   b_qn_gain[layer], b_kn_gain[layer], None)
        ob_lat = attend_blocked(qb_l, jnp.concatenate([kb_c, kb_l], axis=2), jnp.concatenate([vb_c, vb_l], axis=2))

        qc_l, kc_l, vc_l = gqa_qkv(pc_lat, c_qn_gain[layer], c_kn_gain[layer], rope_c)
        qc_c, kc_c, vc_c = gqa_qkv(pc_ctx, c_qn_gain[layer], c_kn_gain[layer], None)
        oc_lat = window_attention_with_ctx(qc_l, kc_l, vc_l, kc_c, vc_c, c_sink[layer])

        o_lat = jnp.concatenate([oa_lat, heads_to_tokens(ob_lat), gqa_out(oc_lat)], axis=-1)
        x = x + m_lat[2] * (o_lat @ w_out[layer])
        if need_ctx:
            oa_ctx = chunk_gmlp(pa_ctx, a_v_gain[layer], a_w_s[layer], a_b_s[layer])
            ob_ctx = softmax_attend(qb_c, kb_c, vb_c)
            oc_ctx = context_sink_attention(qc_c, kc_c, vc_c, c_sink[layer])
            o_ctx = jnp.concatenate([oa_ctx, heads_to_tokens(ob_ctx), gqa_out(oc_ctx)], axis=-1)
            ctx = ctx + m_ctx[2] * (o_ctx @ w_out[layer])

        g_lat = modulate(rms_norm(x, norm2_gain[layer]), m_lat[3], m_lat[4])
        x = x + m_lat[5] * peer_ffn(g_lat, peer_w_q[layer], peer_subkeys[layer], peer_u[layer], peer_v[layer])
        if need_ctx:
            g_ctx = modulate(rms_norm(ctx, norm2_gain[layer]), m_ctx[3], m_ctx[4])
            ctx = ctx + m_ctx[5] * peer_ffn(g_ctx, peer_w_q[layer], peer_subkeys[layer], peer_u[layer], peer_v[layer])

    return x
```

```python
import contextlib
import numpy as np
import ml_dtypes
import concourse.bass as bass
import concourse.mybir as mybir
from concourse.bass_utils import run_bass_kernel_spmd

F32 = mybir.dt.float32
BF16 = mybir.dt.bfloat16
ALU = mybir.AluOpType
AF = mybir.ActivationFunctionType
AX = mybir.AxisListType
EPS = 1e-6


def make_cfg(D=4096, SEQ=2048, CTX=256, DEPTH=2, BATCH=4):
    c = dict(D=D, SEQ=SEQ, CTX=CTX, DEPTH=DEPTH, BATCH=BATCH)
    NS = D // 128
    c['A_H'] = NS // 4
    c['B_H'] = (NS - c['A_H']) // 2
    c['C_H'] = NS - c['A_H'] - c['B_H']
    c['C_KV'] = c['C_H'] // 3
    c['A_W'] = c['A_H'] * 128
    c['Q_LORA'] = 1536
    c['KV_LORA'] = 512
    c['P_A'] = 2 * c['A_W']
    c['P_B'] = 1536 + 512 + 64
    c['P_C'] = (c['C_H'] + 2 * c['C_KV']) * 128
    c['P_IN'] = c['P_A'] + c['P_B'] + c['P_C']
    c['DC'] = D // 128
    c['NT'] = (CTX + SEQ) // 128
    c['NCT'] = CTX // 128
    return c


class Buf:
    def __init__(self, name):
        self.name = name
        self.last_w = None
        self.readers = []
        self.dsem = None
        self.dcount = 0


class Eng:
    def __init__(self, name, eng, sem):
        self.name, self.eng, self.sem = name, eng, sem
        self.count = 0
        self.seen = {}


class Tracker:
    def __init__(self, nc, stack):
        self.nc = nc
        self.stack = stack
        self.engs = {}
        for name, e in (('pe', nc.tensor), ('dve', nc.vector), ('act', nc.scalar), ('pool', nc.gpsimd),
                        ('sp', nc.sync)):
            sem = stack.enter_context(nc.semaphore('prog_' + name))
            self.engs[name] = Eng(name, e, sem)
        self.nbuf = 0
        self.dma_toks = {}
        self.free_dsems = []
        self.phase_bufs = [[]]
        self.ndsem = 0

    def push_phase(self):
        self.phase_bufs.append([])

    def pop_phase(self):
        for b in self.phase_bufs.pop():
            if b.dsem is not None:
                self.free_dsems.append((b.dsem, b.dcount, b.dkey))
                b.dsem = None

    def barrier(self):
        toks = [(E.name, E.sem, E.count) for E in self.engs.values() if E.count > 0] + list(self.dma_toks.values())
        for E in self.engs.values():
            for t in toks:
                if t[0] != E.name:
                    self._wait(E, t)

    def buf(self, name=None):
        self.nbuf += 1
        b = Buf(name or f'b{self.nbuf}')
        self.phase_bufs[-1].append(b)
        return b

    def _wait(self, E, tok):
        if tok is None:
            return
        key, sem, cnt = tok
        if key == E.name and E.name in ('pe',):
            return
        if E.seen.get(key, 0) >= cnt:
            return
        E.eng.wait_ge(sem, cnt)
        E.seen[key] = cnt

    def need(self, E, reads=(), writes=()):
        E = self.engs[E] if isinstance(E, str) else E
        for b in reads:
            self._wait(E, b.last_w)
        for b in writes:
            self._wait(E, b.last_w)
            for r in b.readers:
                self._wait(E, r)

    def done(self, E, ins, reads=(), writes=()):
        E = self.engs[E] if isinstance(E, str) else E
        E.count += 1
        ins.then_inc(E.sem, 1)
        tok = (E.name, E.sem, E.count)
        for b in reads:
            b.readers.append(tok)
            if len(b.readers) > 24:
                b.readers = b.readers[-24:]
        for b in writes:
            b.last_w = tok
            b.readers = []
        return tok

    def op(self, E, fn, reads=(), writes=()):
        Eo = self.engs[E]
        self.need(Eo, reads, writes)
        ins = fn(Eo.eng)
        return self.done(Eo, ins, reads, writes)

    def dma(self, Q, out, in_, reads=(), writes=(), semowner=None, **kw):
        Eo = self.engs[Q]
        self.need(Eo, reads, writes)
        ow = semowner
        if ow.dsem is None:
            if self.free_dsems:
                ow.dsem, ow.dcount, ow.dkey = self.free_dsems.pop()
            else:
                self.ndsem += 1
                ow.dkey = f'd{self.ndsem}'
                ow.dsem = self.stack.enter_context(self.nc.semaphore(ow.dkey))
                ow.dcount = 0
        ins = Eo.eng.dma_start(out=out, in_=in_, **kw)
        ow.dcount += 16
        ins.then_inc(ow.dsem, 16)
        tok = (ow.dkey, ow.dsem, ow.dcount)
        self.dma_toks[tok[0]] = tok
        for b in reads:
            b.readers.append(tok)
        for b in writes:
            b.last_w = tok
            b.readers = []
        return tok

    def wait_all(self, E, toks):
        Eo = self.engs[E]
        for t in toks:
            self._wait(Eo, t)


class Ring:
    def __init__(self, tr, stack, nc, name, n, shape, dtype):
        self.n = n
        self.t = stack.enter_context(nc.sbuf_tensor(name, [shape[0], n] + list(shape[1:]), dtype))
        self.bufs = [tr.buf(f'{name}{i}') for i in range(n)]
        self.i = 0

    def next(self):
        k = self.i % self.n
        self.i += 1
        return self.t[:, k], self.bufs[k]


def pieces(n, step=512):
    return [(i, min(step, n - i)) for i in range(0, n, step)]


def build(cfg, debug=None):
    D, SEQ, CTX, DEPTH = cfg['D'], cfg['SEQ'], cfg['CTX'], cfg['DEPTH']
    DC, NT, NCT = cfg['DC'], cfg['NT'], cfg['NCT']
    A_H, B_H, C_H, C_KV, A_W = cfg['A_H'], cfg['B_H'], cfg['C_H'], cfg['C_KV'], cfg['A_W']
    P_IN = cfg['P_IN']
    NTOK = CTX + SEQ
    L = DEPTH
    QW, KVW = B_H * 192, B_H * 256
    oB, oC = 2 * A_W, 2 * A_W + 2112
    nc = bass.Bass("TRN2", target_bir_lowering=False)

    def din(name, shape):
        return nc.dram_tensor(name, list(shape), F32, kind="ExternalInput")
    x_in = din("x", [SEQ, D]); ctx_in = din("ctx", [CTX, D]); c_in = din("c", [DC, 128]); cc_in = din("c_ctx", [DC, 128])
    w_mod = din("w_mod", [L, D, 6 * D]); b_mod = din("b_mod", [L, 6 * D])
    g1 = din("norm1_gain", [L, DC, 128]); g2 = din("norm2_gain", [L, DC, 128])
    w_in = din("w_in", [L, D, P_IN]); a_vg = din("a_v_gain", [L, A_W]); a_ws = din("a_w_s", [L, 128, 128]); a_bs = din("a_b_s", [L, 128])
    bqg = din("b_q_gain", [L, 1536]); bkvg = din("b_kv_gain", [L, 512]); wuq = din("b_w_uq", [L, 1536, QW]); wukv = din("b_w_ukv", [L, 512, KVW])
    bqn = din("b_qn_gain", [L, 192]); bkn = din("b_kn_gain", [L, 192]); cqn = din("c_qn_gain", [L, 128]); ckn = din("c_kn_gain", [L, 128])
    csink = din("c_sink", [L, C_H]); w_out = din("w_out", [L, D, D]); pwq = din("peer_w_q", [L, D, 2048])
    psk = din("peer_subkeys", [L, 16, 128, 128]); pu = din("peer_u", [L, 16384, D]); pvt = din("peer_v", [L, 16384, D])
    ropeB = din("ropeB", [SEQ, 2, 64]); ropeC = din("ropeC", [SEQ, 2, 128])
    masks = din("masks", [2, 128, 128])
    out = nc.dram_tensor("out", [SEQ, D], F32, kind="ExternalOutput")

    def dscr(name, shape, dt=BF16):
        return nc.dram_tensor(name, list(shape), dt)
    xres = dscr("xres", [NTOK, D], F32)
    win_b = dscr("win_b", [D, P_IN]); wout_b = dscr("wout_b", [D, D]); wq_b = dscr("wq_b", [D, 2048])
    wuq_b = dscr("wuq_b", [1536, QW]); wukv_b = dscr("wukv_b", [512, KVW])
    put_b = dscr("put_b", [128, 128, D])
    pv_b = dscr("pv_b", [16384, D])
    mod_d = dscr("mod_d", [2, 6 * D], F32)
    qBT = dscr("qBT", [B_H, 192, NTOK]); kBT = dscr("kBT", [B_H, 192, NTOK]); vB = dscr("vB", [NTOK, B_H * 128])
    qCT = dscr("qCT", [C_H, 128, NTOK]); kCT = dscr("kCT", [C_KV, 128, NTOK]); vC = dscr("vC", [NTOK, C_KV * 128])
    o_d = dscr("o_d", [NTOK, D])
    dbg = nc.dram_tensor("dbg", list(debug[1]), F32, kind="ExternalOutput") if (debug and debug[0] == 'ptile') else None
    SCR = dict(qBT=qBT, kBT=kBT, vB=vB, qCT=qCT, kCT=kCT, vC=vC, o_d=o_d, xres=xres, mod_d=mod_d)
    dumps = []
    if debug and debug[0] == 'dram':
        for nm in debug[1]:
            tns = SCR[nm]
            v2 = tns.ap() if len(tns.shape) == 2 else tns.ap().rearrange("h w t -> (h w) t")
            od = nc.dram_tensor("dbg_" + nm, list(v2.shape), tns.dtype, kind="ExternalOutput")
            dumps.append((v2, od))

    with contextlib.ExitStack() as top:
        tr = Tracker(nc, top)
        top.enter_context(nc.Block())
        dq = ['sp', 'pool']
        dqi = [0]

        def Q():
            dqi[0] += 1
            return dq[dqi[0] % 2]
        B_xres = [tr.buf(f'xres{t}') for t in range(NT)]
        B_w = {n: tr.buf('W' + n) for n in ('win', 'wout', 'wq', 'wuq', 'wukv', 'put', 'pv')}
        B_mod = tr.buf('modd')
        B_qkv = tr.buf('qkv')
        B_o = [tr.buf(f'o{t}') for t in range(NT)]
        B_out = tr.buf('out')

        @contextlib.contextmanager
        def phase():
            tr.push_phase()
            with contextlib.ExitStack() as st_:
                yield st_
                tr.barrier()
            tr.pop_phase()

        def sbt(st, name, shape, dt=F32):
            return st.enter_context(nc.sbuf_tensor(name, list(shape), dt)), tr.buf(name)

        def pst(st, name, shape, dt=F32):
            return st.enter_context(nc.psum_tensor(name, list(shape), dt)), tr.buf(name)

        idf, b_idf = sbt(top, "idf", [128, 128]); idb, b_idb = sbt(top, "idb", [128, 128], BF16)
        tr.op('pool', lambda e: e.memset(idf[:], 0.0), writes=[b_idf])
        tr.op('pool', lambda e: e.affine_select(out=idf[:], in_=idf[:], pattern=[[-1, 128]], compare_op=ALU.not_equal,
                                                fill=1.0, base=0, channel_multiplier=1), reads=[b_idf], writes=[b_idf])
        tr.op('dve', lambda e: e.tensor_copy(out=idb[:], in_=idf[:]), reads=[b_idf], writes=[b_idb])
        mk, b_mk = sbt(top, "mk", [128, 2, 128], BF16)
        with phase() as st:
            mf, b_mf = sbt(st, "mf", [128, 2, 128])
            tr.dma('sp', mf[:], masks.ap().rearrange("m p j -> p m j"), writes=[b_mf], semowner=b_mf)
            tr.op('dve', lambda e: e.tensor_copy(out=mk[:], in_=mf[:]), reads=[b_mf], writes=[b_mk])

        BK = []
        for i in range(4):
            BK.append(pst(top, f"bank{i}", [128, 512]))
        tr_in, b_trin = sbt(top, "tr_in", [128, 128]); tr_ps, b_trps = BK[0][0][:, 0:128], BK[0][1]
        MMP = [BK[1], BK[2]]

        def transpose_rows(st, src2d, nrows, name):
            t_o, b_o = sbt(st, name, [128, nrows])
            tr.dma('sp', tr_in[0:nrows, :], src2d, writes=[b_trin], semowner=b_trin)
            tr.op('pe', lambda e: e.transpose(tr_ps[:, 0:nrows], tr_in[0:nrows, :], idf[0:nrows, 0:nrows]), reads=[b_trin, b_idf], writes=[b_trps])
            tr.op('dve', lambda e: e.tensor_copy(out=t_o[:], in_=tr_ps[:, 0:nrows]), reads=[b_trps], writes=[b_o])
            return t_o, b_o

        with phase() as st:
            ring = Ring(tr, st, nc, "xcp", 3, [128, D], F32)
            for t in range(NT):
                src = ctx_in[t * 128:(t + 1) * 128, :] if t < NCT else x_in[(t - NCT) * 128:(t - NCT + 1) * 128, :]
                ap, b = ring.next()
                q = Q()
                tr.dma(q, ap, src, writes=[b], semowner=b)
                tr.dma(q, xres[t * 128:(t + 1) * 128, :], ap, reads=[b], writes=[B_xres[t]], semowner=b)

        scT, b_scT = sbt(top, "scT", [128, DC, 2])
        with phase() as st:
            for r, src in enumerate((c_in, cc_in)):
                t_o, b_o = transpose_rows(st, src[:, :], DC, f"cT{r}")
                tr.op('act', lambda e: e.activation(out=scT[:, :, r], in_=t_o[:], func=AF.Silu), reads=[b_o, b_scT], writes=[b_scT])

        cast_engs = ['dve', 'act', 'pool']
        ci = [0]

        def cast(out_ap, in_ap, reads, writes):
            ci[0] += 1
            en = cast_engs[ci[0] % 3]
            if en == 'act':
                return tr.op('act', lambda e: e.copy(out=out_ap, in_=in_ap), reads=reads, writes=writes)
            return tr.op(en, lambda e: e.tensor_copy(out=out_ap, in_=in_ap), reads=reads, writes=writes)

        def cast_copy(st, src, dst, rows, cols, bdst, tag):
            rf = Ring(tr, st, nc, tag + "f", 4, [128, 2048], F32)
            rb = Ring(tr, st, nc, tag + "b", 4, [128, 2048], BF16)
            for r0 in range(0, rows, 128):
                for c0, cw in pieces(cols, 2048):
                    fa, fb = rf.next(); ba, bb = rb.next()
                    q = Q()
                    tr.dma(q, fa[:, 0:cw], src[r0:r0 + 128, c0:c0 + cw], writes=[fb], semowner=fb)
                    cast(ba[:, 0:cw], fa[:, 0:cw], [fb], [bb])
                    tr.dma(q, dst[r0:r0 + 128, c0:c0 + cw], ba[:, 0:cw], reads=[bb], semowner=bb)

        def prep_tables(l):
            with phase() as st:
                cast_copy(st, pvt[l], pv_b, 16384, D, B_w['pv'], f"cwv{l}")
            with phase() as st:
                rf = Ring(tr, st, nc, f"uf{l}", 2, [128, D], F32)
                rb = Ring(tr, st, nc, f"ub{l}", 2, [128, DC, 128], BF16)
                pr = [pst(st, f"up{l}_{i}", [128, 4, 128]) for i in range(2)]
                k = 0
                for ec in range(128):
                    ba, bb = rb.next()
                    fa, fb = rf.next()
                    tr.dma(Q(), fa, pu[l, ec * 128:(ec + 1) * 128, :], writes=[fb], semowner=fb)
                    for c4 in range(0, DC, 4):
                        pp, bp = pr[k % 2]; k += 1
                        def tp(e, pp=pp, fa=fa, c4=c4):
                            for i in range(4):
                                ins = e.transpose(pp[:, i, :], fa[:, (c4 + i) * 128:(c4 + i + 1) * 128], idf[:])
                            return ins
                        tr.op('pe', tp, reads=[fb, b_idf], writes=[bp])
                        evac_copy(ba[:, c4:c4 + 4, :], bb, pp[:, :, :], bp)
                    tr.dma(Q(), put_b[ec].rearrange("p (k e) -> p k e", e=128), ba, reads=[bb], semowner=bb)

        def rstd_from_ss(ss_ap, b_ss, n):
            tr.op('dve', lambda e: e.tensor_scalar(out=ss_ap, in0=ss_ap, scalar1=1.0 / n, scalar2=EPS, op0=ALU.mult, op1=ALU.add), reads=[b_ss], writes=[b_ss])
            tr.op('act', lambda e: e.activation(out=ss_ap, in_=ss_ap, func=AF.Sqrt), reads=[b_ss], writes=[b_ss])
            tr.op('dve', lambda e: e.reciprocal(out=ss_ap, in_=ss_ap), reads=[b_ss], writes=[b_ss])

        def bload(st, name, src1d, n):
            t, b = sbt(st, name, [128, n])
            tr.dma('sp', t[:], src1d.partition_broadcast(128), writes=[b], semowner=b)
            return t, b

        def phase_mod(l):
            with phase() as st:
                wr = Ring(tr, st, nc, f"wm{l}", 2, [128, DC, 512], F32)
                pm = [(MMP[i][0][0:2, :], MMP[i][1]) for i in range(2)]
                br = Ring(tr, st, nc, f"bm{l}", 2, [2, 512], F32)
                mr = Ring(tr, st, nc, f"mr{l}", 2, [2, 512], F32)
                for i, (c0, cw) in enumerate(pieces(6 * D)):
                    wt, bw = wr.next()
                    tr.dma(Q(), wt, w_mod[l, :, c0:c0 + cw].rearrange("(k p) c -> p k c", p=128), writes=[bw], semowner=bw)
                    bt, bb = br.next()
                    tr.dma('sp', bt, b_mod[l, c0:c0 + cw].partition_broadcast(2), writes=[bb], semowner=bb)
                    pp, bp = pm[i % 2]
                    def mm(e, pp=pp, wt=wt):
                        for k in range(DC):
                            ins = e.matmul(pp[:, :], scT[:, k, :], wt[:, k, :], start=(k == 0), stop=(k == DC - 1))
                        return ins
                    tr.op('pe', mm, reads=[bw, b_scT], writes=[bp])
                    mt, bmt = mr.next()
                    tr.op('dve', lambda e, mt=mt, pp=pp, bt=bt: e.tensor_tensor(out=mt, in0=pp[:, :], in1=bt, op=ALU.add), reads=[bp, bb], writes=[bmt])
                    tr.dma('sp', mod_d[:, c0:c0 + cw], mt, reads=[bmt], semowner=bmt)

        def load_layer_consts(st, l):
            K = {}
            for nm in ("1", "2"):
                K['A' + nm], K['bA' + nm] = sbt(st, f"A{nm}_{l}", [128, DC, 2])
                K['B' + nm], K['bB' + nm] = sbt(st, f"B{nm}_{l}", [128, DC, 2])
            with phase() as s2:
                mT = {}
                for i in (0, 1, 3, 4):
                    for r in range(2):
                        mT[i, r] = transpose_rows(s2, mod_d[r, i * D:(i + 1) * D].rearrange("(c p) -> c p", p=128), DC, f"mT{l}_{i}{r}")
                g1T = transpose_rows(s2, g1[l], DC, f"g1T{l}")
                g2T = transpose_rows(s2, g2[l], DC, f"g2T{l}")
                for nm, gT, isc, ish in (("1", g1T, 1, 0), ("2", g2T, 4, 3)):
                    A, bA, Bt, bB = K['A' + nm], K['bA' + nm], K['B' + nm], K['bB' + nm]
                    for r in range(2):
                        tr.op('dve', lambda e, A=A, r=r, gT=gT, isc=isc: e.scalar_tensor_tensor(out=A[:, :, r], in0=mT[isc, r][0][:], scalar=1.0, in1=gT[0][:], op0=ALU.add, op1=ALU.mult),
                              reads=[mT[isc, r][1], gT[1], bA], writes=[bA])
                        tr.op('dve', lambda e, Bt=Bt, r=r, ish=ish: e.tensor_copy(out=Bt[:, :, r], in_=mT[ish, r][0][:]), reads=[mT[ish, r][1], bB], writes=[bB])
            return K

        def ln_temps(st, tag):
            T = {}
            T['xt'], T['bxt'] = sbt(st, tag + "x", [128, D])
            T['jk'], T['bjk'] = sbt(st, tag + "j", [128, D], BF16)
            T['ss'], T['bss'] = sbt(st, tag + "s", [128, 1])
            T['pr'] = [pst(st, f"{tag}p{i}", [128, 4, 128], BF16) for i in range(2)]
            return T

        def ln_mod_T(T, t, A, bA, Bm, bB, hT, b_hT):
            r = 1 if t < NCT else 0
            xt, bxt, jk, bjk, ss, bss, pr = T['xt'], T['bxt'], T['jk'], T['bjk'], T['ss'], T['bss'], T['pr']
            tr.dma(Q(), xt[:], xres[t * 128:(t + 1) * 128, :], reads=[B_xres[t]], writes=[bxt], semowner=bxt)
            tr.op('act', lambda e: e.activation(out=jk[:], in_=xt[:], func=AF.Square, accum_out=ss[:]), reads=[bxt], writes=[bjk, bss])
            rstd_from_ss(ss[:], bss, D)
            tr.op('dve', lambda e: e.tensor_scalar(out=jk[:], in0=xt[:], scalar1=ss[:, 0:1], scalar2=None, op0=ALU.mult), reads=[bxt, bss], writes=[bjk])
            for i, c4 in enumerate(range(0, DC, 4)):
                pp, bp = pr[i % 2]
                def tp(e, pp=pp, c4=c4):
                    for j in range(4):
                        ins = e.transpose(pp[:, j, :], jk[:, (c4 + j) * 128:(c4 + j + 1) * 128], idb[:])
                    return ins
                tr.op('pe', tp, reads=[bjk, b_idb], writes=[bp])
                for j in range(4):
                    c = c4 + j
                    en = 'dve' if j % 2 == 0 else 'pool'
                    if en == 'pool':
                        en = 'dve'
                    tr.op(en, lambda e, pp=pp, j=j, c=c: e.tensor_scalar(out=hT[:, c, :], in0=pp[:, j, :], scalar1=A[:, c, r:r + 1], scalar2=Bm[:, c, r:r + 1], op0=ALU.mult, op1=ALU.add),
                          reads=[bp, bA, bB, b_hT], writes=[b_hT])
            return xt, bxt

        class Lin:
            def __init__(self, st, KCmax, tag, pw=512, depth=2):
                self.pw = pw
                self.wr = Ring(tr, st, nc, tag + "w", depth, [128, KCmax, pw], BF16)
                self.ps = [(MMP[i][0], MMP[i][1]) for i in range(2)]
                self.i = 0

            def run(self, actT, b_act, KC, w_d, ncols, evac, col0=0):
                for c0, cw in pieces(ncols, self.pw):
                    wt, bw = self.wr.next()
                    tr.dma(Q(), wt[:, 0:KC, 0:cw], w_d[0:KC * 128, col0 + c0:col0 + c0 + cw].rearrange("(k p) c -> p k c", p=128), writes=[bw], semowner=bw)
                    pp, bp = self.ps[self.i % 2]; self.i += 1
                    def mm(e, pp=pp, wt=wt, cw=cw):
                        for k in range(KC):
                            ins = e.matmul(pp[:, 0:cw], actT[:, k, :], wt[:, k, 0:cw], start=(k == 0), stop=(k == KC - 1))
                        return ins
                    tr.op('pe', mm, reads=[bw, b_act], writes=[bp])
                    evac(c0, cw, pp, bp)

        def lin_multi(lin, acts, KC, w_d, ncols):
            for c0, cw in pieces(ncols, lin.pw):
                wt, bw = lin.wr.next()
                tr.dma(Q(), wt[:, 0:KC, 0:cw], w_d[0:KC * 128, c0:c0 + cw].rearrange("(k p) c -> p k c", p=128), writes=[bw], semowner=bw)
                for (actT, b_act, evac) in acts:
                    pp, bp = lin.ps[lin.i % 2]; lin.i += 1
                    def mm(e, pp=pp, wt=wt, cw=cw, actT=actT):
                        for k in range(KC):
                            ins = e.matmul(pp[:, 0:cw], actT[:, k, :], wt[:, k, 0:cw], start=(k == 0), stop=(k == KC - 1))
                        return ins
                    tr.op('pe', mm, reads=[bw, b_act], writes=[bp])
                    evac(c0, cw, pp, bp)

        evi = [0]

        def evac_copy(out_ap, b_out, pp_ap, bp):
            evi[0] += 1
            if evi[0] % 2:
                tr.op('dve', lambda e: e.tensor_copy(out=out_ap, in_=pp_ap), reads=[bp, b_out], writes=[b_out])
            else:
                tr.op('act', lambda e: e.copy(out=out_ap, in_=pp_ap), reads=[bp, b_out], writes=[b_out])

        def transposes_bf16(st_ps, src, b_src, nchunks, dst, b_dst, width=128):
            for c4 in range(0, nchunks, 4):
                pp, bp = st_ps[(c4 // 4) % 2]
                n = min(4, nchunks - c4)
                def tp(e, pp=pp, c4=c4, n=n):
                    for j in range(n):
                        ins = e.transpose(pp[0:width, j, :], src[:, (c4 + j) * width:(c4 + j + 1) * width], idb[:])
                    return ins
                tr.op('pe', tp, reads=[b_src, b_idb], writes=[bp])
                evac_copy(dst[0:width, c4:c4 + n, :], b_dst, pp[0:width, 0:n, :], bp)

        def mixer_prep(l, K, need_ctx):
            with phase() as st:
                PW = 512 if DC <= 16 else 256
                hT, b_hT = sbt(st, f"hT{l}", [128, DC, 128], BF16)
                LT = ln_temps(st, f"ln{l}")
                lin = Lin(st, max(DC, 12), f"l1{l}", PW)
                pt, bpt = sbt(st, f"ptile{l}", [128, P_IN])
                avg_b = bload(st, f"avg{l}", a_vg[l], A_W); qg_b = bload(st, f"qg{l}", bqg[l], 1536); kvg_b = bload(st, f"kvg{l}", bkvg[l], 512)
                bqn_b = bload(st, f"bqn{l}", bqn[l], 192); bkn_b = bload(st, f"bkn{l}", bkn[l], 192)
                cqn_b = bload(st, f"cqn{l}", cqn[l], 128); ckn_b = bload(st, f"ckn{l}", ckn[l], 128)
                wsT_f = transpose_rows(st, a_ws[l], 128, f"wsT{l}")
                wsT, b_wsT = sbt(st, f"wsTb{l}", [128, 128], BF16)
                tr.op('dve', lambda e: e.tensor_copy(out=wsT[:], in_=wsT_f[0][:]), reads=[wsT_f[1]], writes=[b_wsT])
                bs_c, b_bs = sbt(st, f"bsc{l}", [128, 1])
                tr.dma('sp', bs_c[:], a_bs[l].rearrange("(p o) -> p o", o=1), writes=[b_bs], semowner=b_bs)
                SW = max(B_H * 192, A_W)
                S1, b_S1 = sbt(st, f"S1{l}", [128, SW]); S2, b_S2 = sbt(st, f"S2{l}", [128, SW]); S3, b_S3 = sbt(st, f"S3{l}", [128, SW])
                ssH, b_ssH = sbt(st, f"ssH{l}", [128, 16]); ss1, b_ss1 = sbt(st, f"ss1{l}", [128, 1])
                qf, b_qf = sbt(st, f"qf{l}", [128, B_H * 192]); kvf, b_kvf = sbt(st, f"kvf{l}", [128, B_H * 256])
                NB, b_NB = sbt(st, f"NB{l}", [128, SW], BF16); hTt, b_hTt = sbt(st, f"hTt{l}", [128, 2 * B_H, 128], BF16)
                cqb, b_cqb = sbt(st, f"cqb{l}", [128, 1536], BF16); cqT, b_cqT = sbt(st, f"cqT{l}", [128, 12, 128], BF16)
                vb, b_vb = sbt(st, f"vb{l}", [128, B_H * 128], BF16); ot, b_ot = sbt(st, f"ot{l}", [128, A_W], BF16)
                tbB, b_tbB = sbt(st, f"tbB{l}", [128, 2, 64]); tbC, b_tbC = sbt(st, f"tbC{l}", [128, 2, 128])
                tps = [pst(st, f"tp{l}_{i}", [128, 4, 128], BF16) for i in range(2)]

                def norm_heads(src3, b_src, H, W, gain3, b_gain, dst3, b_dst):
                    sq = S1[:, 0:H * W].rearrange("p (h w) -> p h w", w=W)
                    tr.op('pool', lambda e: e.tensor_tensor(out=sq, in0=src3, in1=src3, op=ALU.mult), reads=[b_src], writes=[b_S1])
                    tr.op('dve', lambda e: e.tensor_reduce(out=ssH[:, 0:H], in_=sq, axis=AX.X, op=ALU.add), reads=[b_S1], writes=[b_ssH])
                    rstd_from_ss(ssH[:, 0:H], b_ssH, W)
                    tr.op('dve', lambda e: e.tensor_tensor(out=sq, in0=src3, in1=ssH[:, 0:H].unsqueeze(2).broadcast_to([128, H, W]), op=ALU.mult), reads=[b_src, b_ssH], writes=[b_S1])
                    tr.op('dve', lambda e: e.tensor_tensor(out=dst3, in0=sq, in1=gain3, op=ALU.mult), reads=[b_S1, b_gain, b_dst], writes=[b_dst])

                def rope3(x3, b_x, H, W, tab, b_tab):
                    hw = W // 4
                    t1 = S1[:, 0:H * W].rearrange("p (h w) -> p h w", w=W); t2 = S3[:, 0:H * W].rearrange("p (h w) -> p h w", w=W)
                    tr.op('dve', lambda e: e.tensor_tensor(out=t1, in0=x3, in1=tab[:, 0, :].unsqueeze(1).broadcast_to([128, H, W]), op=ALU.mult), reads=[b_x, b_tab], writes=[b_S1])
                    for a in range(2):
                        lo = (a * 2 * hw, a * 2 * hw + hw); hi = (a * 2 * hw + hw, (a + 1) * 2 * hw)
                        for (o, i) in ((lo, hi), (hi, lo)):
                            tr.op('pool', lambda e, o=o, i=i: e.tensor_tensor(out=t2[:, :, o[0]:o[1]], in0=x3[:, :, i[0]:i[1]],
                                                                             in1=tab[:, 1, o[0]:o[1]].unsqueeze(1).broadcast_to([128, H, hw]), op=ALU.mult),
                                  reads=[b_x, b_tab, b_S3], writes=[b_S3])
                    tr.op('dve', lambda e: e.tensor_tensor(out=x3, in0=t1, in1=t2, op=ALU.add), reads=[b_S1, b_S3, b_x], writes=[b_x])

                def to_T_store(H, W, dstT, tok):
                    items = []
                    for h in range(H):
                        items.append((h * W, 128, h))
                        if W == 192:
                            items.append((h * W + 128, 64, H + h))
                    for g0 in range(0, len(items), 4):
                        grp = items[g0:g0 + 4]
                        pp, bp = tps[(g0 // 4) % 2]
                        def tp(e, pp=pp, grp=grp):
                            for j, (c0, w, slot) in enumerate(grp):
                                ins = e.transpose(pp[0:w, j, :], NB[:, c0:c0 + w], idb[:])
                            return ins
                        tr.op('pe', tp, reads=[b_NB, b_idb], writes=[bp])
                        for j, (c0, w, slot) in enumerate(grp):
                            evac_copy(hTt[0:w, slot, :], b_hTt, pp[0:w, j, :], bp)
                    tr.dma(Q(), dstT[:, 0:128, tok].rearrange("h d t -> d h t"), hTt[:, 0:H, :], reads=[b_hTt], semowner=b_hTt)
                    if W == 192:
                        tr.dma(Q(), dstT[:, 128:192, tok].rearrange("h d t -> d h t"), hTt[0:64, H:2 * H, :], reads=[b_hTt], semowner=b_hTt)

                def rms_rows(src, b_src, n, gain_b, dstb, b_dst):
                    tr.op('act', lambda e: e.activation(out=dstb, in_=src, func=AF.Square, accum_out=ss1[:]), reads=[b_src, b_dst], writes=[b_dst, b_ss1])
                    rstd_from_ss(ss1[:], b_ss1, n)
                    tr.op('dve', lambda e: e.scalar_tensor_tensor(out=dstb, in0=src, scalar=ss1[:, 0:1], in1=gain_b[0][:, 0:n], op0=ALU.mult, op1=ALU.mult),
                          reads=[b_src, b_ss1, gain_b[1], b_dst], writes=[b_dst])

                for t in range(NT):
                    lat = t >= NCT
                    tok = slice(t * 128, (t + 1) * 128)
                    ln_mod_T(LT, t, K['A1'], K['bA1'], K['B1'], K['bB1'], hT, b_hT)
                    def ev_in(c0, cw, pp, bp):
                        if c0 < 2 * A_W:
                            tr.op('act', lambda e: e.activation(out=pt[:, c0:c0 + cw], in_=pp[:, 0:cw], func=AF.Gelu), reads=[bp, bpt], writes=[bpt])
                        else:
                            evac_copy(pt[:, c0:c0 + cw], bpt, pp[:, 0:cw], bp)
                    lin.run(hT, b_hT, DC, win_b, P_IN, ev_in)
                    if debug and debug[0] == 'ptile' and t == debug[2]:
                        tr.dma('sp', dbg[:, :], pt[:, :], reads=[bpt], semowner=bpt)
                    if lat:
                        p0 = (t - NCT) * 128
                        tr.dma('sp', tbB[:], ropeB[p0:p0 + 128], writes=[b_tbB], semowner=b_tbB)
                        tr.dma('sp', tbC[:], ropeC[p0:p0 + 128], writes=[b_tbC], semowner=b_tbC)
                    if lat or need_ctx:
                        vn3 = NB[:, 0:A_W].rearrange("p (h w) -> p h w", w=128)
                        norm_heads(pt[:, A_W:2 * A_W].rearrange("p (h w) -> p h w", w=128), bpt, A_H, 128,
                                   avg_b[0][:, :].rearrange("p (h w) -> p h w", w=128), avg_b[1], vn3, b_NB)
                        for c0, cw in pieces(A_W):
                            pp, bp = MMP[lin.i % 2]; lin.i += 1
                            tr.op('pe', lambda e, pp=pp, c0=c0, cw=cw: e.matmul(pp[:, 0:cw], wsT[:], NB[:, c0:c0 + cw], start=True, stop=True), reads=[b_wsT, b_NB], writes=[bp])
                            tr.op('dve', lambda e, pp=pp, c0=c0, cw=cw: e.scalar_tensor_tensor(out=ot[:, c0:c0 + cw], in0=pp[:, 0:cw], scalar=bs_c[:, 0:1], in1=pt[:, c0:c0 + cw], op0=ALU.add, op1=ALU.mult),
                                  reads=[bp, b_bs, bpt, b_ot], writes=[b_ot])
                        tr.dma(Q(), o_d[tok, 0:A_W], ot[:], reads=[b_ot], semowner=b_ot)
                    rms_rows(pt[:, oB:oB + 1536], bpt, 1536, qg_b, cqb[:], b_cqb)
                    transposes_bf16(tps, cqb, b_cqb, 12, cqT, b_cqT)
                    lin.run(cqT, b_cqT, 12, wuq_b, QW, lambda c0, cw, pp, bp: evac_copy(qf[:, c0:c0 + cw], b_qf, pp[:, 0:cw], bp))
                    q3 = qf[:, :].rearrange("p (h w) -> p h w", w=192); x3 = S2[:, 0:B_H * 192].rearrange("p (h w) -> p h w", w=192)
                    def finish_B(dstT):
                        if lat:
                            rope3(x3[:, :, 128:192], b_S2, B_H, 64, tbB, b_tbB)
                        tr.op('act', lambda e: e.copy(out=NB[:, 0:B_H * 192], in_=S2[:, 0:B_H * 192]), reads=[b_S2, b_NB], writes=[b_NB])
                        to_T_store(B_H, 192, dstT, tok)
                    norm_heads(q3, b_qf, B_H, 192, bqn_b[0][:, :].unsqueeze(1).broadcast_to([128, B_H, 192]), bqn_b[1], x3, b_S2)
                    finish_B(qBT)
                    rms_rows(pt[:, oB + 1536:oB + 2048], bpt, 512, kvg_b, cqb[:, 0:512], b_cqb)
                    transposes_bf16(tps, cqb, b_cqb, 4, cqT, b_cqT)
                    lin.run(cqT, b_cqT, 4, wukv_b, KVW, lambda c0, cw, pp, bp: evac_copy(kvf[:, c0:c0 + cw], b_kvf, pp[:, 0:cw], bp))
                    kv3 = kvf[:, :].rearrange("p (h w) -> p h w", w=256)
                    tr.op('pool', lambda e: e.tensor_copy(out=q3[:, :, 0:128], in_=kv3[:, :, 0:128]), reads=[b_kvf, b_qf], writes=[b_qf])
                    tr.op('pool', lambda e: e.tensor_copy(out=q3[:, :, 128:192], in_=pt[:, oB + 2048:oB + 2112].unsqueeze(1).broadcast_to([128, B_H, 64])), reads=[bpt, b_qf], writes=[b_qf])
                    norm_heads(q3, b_qf, B_H, 192, bkn_b[0][:, :].unsqueeze(1).broadcast_to([128, B_H, 192]), bkn_b[1], x3, b_S2)
                    finish_B(kBT)
                    tr.op('act', lambda e: e.copy(out=vb[:, :].rearrange("p (h w) -> p h w", w=128), in_=kv3[:, :, 128:256]), reads=[b_kvf, b_vb], writes=[b_vb])
                    tr.dma(Q(), vB[tok, :], vb[:, :], reads=[b_vb], semowner=b_vb)
                    for (c_off, H, gb, dstT) in ((oC, C_H, cqn_b, qCT), (oC + C_H * 128, C_KV, ckn_b, kCT)):
                        xc3 = S2[:, 0:H * 128].rearrange("p (h w) -> p h w", w=128)
                        norm_heads(pt[:, c_off:c_off + H * 128].rearrange("p (h w) -> p h w", w=128), bpt, H, 128,
                                   gb[0][:, :].unsqueeze(1).broadcast_to([128, H, 128]), gb[1], xc3, b_S2)
                        if lat:
                            rope3(xc3, b_S2, H, 128, tbC, b_tbC)
                        tr.op('act', lambda e, H=H: e.copy(out=NB[:, 0:H * 128], in_=S2[:, 0:H * 128]), reads=[b_S2, b_NB], writes=[b_NB])
                        to_T_store(H, 128, dstT, tok)
                    vc0 = oC + (C_H + C_KV) * 128
                    tr.op('act', lambda e: e.copy(out=vb[:, 0:C_KV * 128], in_=pt[:, vc0:vc0 + C_KV * 128]), reads=[bpt, b_vb], writes=[b_vb])
                    tr.dma(Q(), vC[tok, :], vb[:, 0:C_KV * 128], reads=[b_vb], semowner=b_vb)

        def attention(l, need_ctx):
            with phase() as st:
                kTn, b_kTn = sbt(st, f"kTn{l}", [128, NTOK], BF16); kTr, b_kTr = sbt(st, f"kTr{l}", [64, NTOK], BF16)
                V, b_V = sbt(st, f"V{l}", [128, NT, 129], BF16)
                tr.op('pool', lambda e: e.memset(V[:, :, 128:129], 1.0), writes=[b_V])
                qTn, b_qTn = sbt(st, f"qTn{l}", [128, 512], BF16); qTr, b_qTr = sbt(st, f"qTr{l}", [64, 512], BF16)
                PTr = Ring(tr, st, nc, f"PT{l}", 2, [128, 640], BF16)
                ob, b_ob = sbt(st, f"ob{l}", [128, 4, 128], BF16)
                rd, b_rd = sbt(st, f"rd{l}", [128, 4])
                es, b_es = bload(st, f"es{l}", csink[l], C_H)
                tr.op('act', lambda e: e.activation(out=es[:], in_=es[:], func=AF.Exp), reads=[b_es], writes=[b_es])
                ACC = [BK[3]] + [pst(st, f"acc{l}_{i}", [128, 512]) for i in range(3)]
                SP2 = pst(st, f"sp2{l}", [128, 512])

                def finish(acc_list, ntile, extra_den, col0, tok0):
                    for i in range(ntile):
                        ac, bac = acc_list[i]
                        if extra_den is None:
                            tr.op('dve', lambda e, ac=ac, i=i: e.reciprocal(out=rd[:, i:i + 1], in_=ac[:, 128:129]), reads=[bac, b_rd], writes=[b_rd])
                        else:
                            tr.op('dve', lambda e, ac=ac, i=i: e.tensor_tensor(out=rd[:, i:i + 1], in0=ac[:, 128:129], in1=extra_den, op=ALU.add), reads=[bac, b_es, b_rd], writes=[b_rd])
                            tr.op('dve', lambda e, i=i: e.reciprocal(out=rd[:, i:i + 1], in_=rd[:, i:i + 1]), reads=[b_rd], writes=[b_rd])
                        tr.op('dve', lambda e, ac=ac, i=i: e.tensor_scalar(out=ob[:, i, :], in0=ac[:, 0:128], scalar1=rd[:, i:i + 1], scalar2=None, op0=ALU.mult), reads=[bac, b_rd, b_ob], writes=[b_ob])
                    tr.dma(Q(), o_d[tok0:tok0 + ntile * 128, col0:col0 + 128].rearrange("(t p) d -> p t d", p=128), ob[:, 0:ntile, :], reads=[b_ob], semowner=b_ob)

                sc_b = 192.0 ** -0.5
                for h in range(B_H):
                    tr.dma(Q(), kTn[:], kBT[h, 0:128, :], writes=[b_kTn], semowner=b_kTn)
                    tr.dma(Q(), kTr[:], kBT[h, 128:192, :], writes=[b_kTr], semowner=b_kTr)
                    tr.dma(Q(), V[:, :, 0:128], vB[:, h * 128:(h + 1) * 128].rearrange("(t p) d -> p t d", p=128), writes=[b_V], semowner=b_V)
                    blocks = [(CTX + q0, min(512, SEQ - q0), list(range(NT))) for q0 in range(0, SEQ, 512)]
                    if need_ctx:
                        blocks.append((0, CTX, list(range(NCT))))
                    for (tok0, nq, ktiles) in blocks:
                        nqt = nq // 128
                        tr.dma(Q(), qTn[:, 0:nq], qBT[h, 0:128, tok0:tok0 + nq], writes=[b_qTn], semowner=b_qTn)
                        tr.dma(Q(), qTr[:, 0:nq], qBT[h, 128:192, tok0:tok0 + nq], writes=[b_qTr], semowner=b_qTr)
                        for ki, kt in enumerate(ktiles):
                            pp, bp = MMP[ki % 2]
                            def mm(e, pp=pp, kt=kt, nq=nq):
                                e.matmul(pp[:, 0:nq], kTn[:, kt * 128:(kt + 1) * 128], qTn[:, 0:nq], start=True, stop=False)
                                return e.matmul(pp[:, 0:nq], kTr[:, kt * 128:(kt + 1) * 128], qTr[:, 0:nq], start=False, stop=True)
                            tr.op('pe', mm, reads=[b_kTn, b_kTr, b_qTn, b_qTr], writes=[bp])
                            PT, b_PT = PTr.next()
                            tr.op('act', lambda e, PT=PT, pp=pp, nq=nq: e.activation(out=PT[:, 0:nq], in_=pp[:, 0:nq], func=AF.Exp, scale=sc_b), reads=[bp], writes=[b_PT])
                            for qi in range(nqt):
                                ac, bac = ACC[qi]
                                tr.op('pe', lambda e, ac=ac, PT=PT, qi=qi, kt=kt, ki=ki, n=len(ktiles): e.matmul(ac[:, 0:129], PT[:, qi * 128:(qi + 1) * 128], V[:, kt, :], start=(ki == 0), stop=(ki == n - 1)),
                                      reads=[b_PT, b_V], writes=[bac])
                        finish(ACC, nqt, None, A_W + h * 128, tok0)

                sc_c = 128.0 ** -0.5
                for g in range(C_KV):
                    tr.dma(Q(), kTn[:], kCT[g, :, :], writes=[b_kTn], semowner=b_kTn)
                    tr.dma(Q(), V[:, :, 0:128], vC[:, g * 128:(g + 1) * 128].rearrange("(t p) d -> p t d", p=128), writes=[b_V], semowner=b_V)
                    for hh in range(3):
                        h = g * 3 + hh
                        col0 = A_W + B_H * 128 + h * 128
                        qtiles = list(range(NCT, NT)) + (list(range(NCT)) if need_ctx else [])
                        for b0 in range(0, len(qtiles), 4):
                            grp = qtiles[b0:b0 + 4]
                            tok0 = grp[0] * 128
                            tr.dma(Q(), qTn[:, 0:len(grp) * 128], qCT[h, :, tok0:tok0 + len(grp) * 128], writes=[b_qTn], semowner=b_qTn)
                            for gi, t in enumerate(grp):
                                if t >= NCT:
                                    keys = [(kt, None) for kt in range(NCT)]
                                    if t - 1 >= NCT:
                                        keys.append((t - 1, 0))
                                    keys.append((t, None))
                                    if t + 1 < NT:
                                        keys.append((t + 1, 1))
                                else:
                                    keys = [(kt, None) for kt in range(NCT)]
                                nk = len(keys)
                                pp, bp = (MMP[0] if gi % 2 == 0 else MMP[1])
                                p2, bp2 = SP2
                                def mm(e, pp=pp, p2=p2, keys=keys, gi=gi):
                                    for j, (kt, _) in enumerate(keys):
                                        dst = pp[:, j * 128:(j + 1) * 128] if j < 4 else p2[:, 0:128]
                                        ins = e.matmul(dst, kTn[:, kt * 128:(kt + 1) * 128], qTn[:, gi * 128:(gi + 1) * 128], start=True, stop=True)
                                    return ins
                                tr.op('pe', mm, reads=[b_kTn, b_qTn], writes=[bp] + ([bp2] if nk > 4 else []))
                                PT, b_PT = PTr.next()
                                n1 = min(nk, 4)
                                tr.op('act', lambda e, PT=PT, pp=pp, n1=n1: e.activation(out=PT[:, 0:n1 * 128], in_=pp[:, 0:n1 * 128], func=AF.Exp, scale=sc_c), reads=[bp], writes=[b_PT])
                                if nk > 4:
                                    tr.op('act', lambda e, PT=PT, p2=p2: e.activation(out=PT[:, 512:640], in_=p2[:, 0:128], func=AF.Exp, scale=sc_c), reads=[bp2, b_PT], writes=[b_PT])
                                for j, (kt, m) in enumerate(keys):
                                    if m is not None:
                                        tr.op('dve', lambda e, PT=PT, j=j, m=m: e.tensor_tensor(out=PT[:, j * 128:(j + 1) * 128], in0=PT[:, j * 128:(j + 1) * 128], in1=mk[:, m, :], op=ALU.mult),
                                              reads=[b_PT, b_mk], writes=[b_PT])
                                ac, bac = ACC[gi]
                                def pv(e, ac=ac, PT=PT, keys=keys):
                                    for j, (kt, _) in enumerate(keys):
                                        ins = e.matmul(ac[:, 0:129], PT[:, j * 128:(j + 1) * 128], V[:, kt, :], start=(j == 0), stop=(j == len(keys) - 1))
                                    return ins
                                tr.op('pe', pv, reads=[b_PT, b_V], writes=[bac])
                            finish(ACC, len(grp), es[:, h:h + 1], col0, tok0)

        def out_proj(l, need_ctx):
            with phase() as st:
                gate = [bload(st, f"g2_{l}_{r}", mod_d[r, 2 * D:3 * D], D) for r in range(2 if need_ctx else 1)]
                lin = Lin(st, DC, f"lo{l}", 512, depth=3)
                NP = 2
                otl = [sbt(st, f"otl{l}_{j}", [128, D], BF16) for j in range(NP)]; oT = [sbt(st, f"oT{l}_{j}", [128, DC, 128], BF16) for j in range(NP)]
                xts = [sbt(st, f"xo{l}_{j}", [128, D]) for j in range(NP)]; tmps = [sbt(st, f"to{l}_{j}", [128, 512]) for j in range(NP)]
                tps = [pst(st, f"tpo{l}_{i}", [128, 4, 128], BF16) for i in range(2)]
                tl = (list(range(NT)) if need_ctx else list(range(NCT, NT)))
                for t0 in range(0, len(tl), NP):
                    acts = []
                    for j, t in enumerate(tl[t0:t0 + NP]):
                        r = 1 if t < NCT else 0
                        tok = slice(t * 128, (t + 1) * 128)
                        tr.dma(Q(), otl[j][0][:], o_d[tok, :], writes=[otl[j][1]], semowner=otl[j][1])
                        tr.dma(Q(), xts[j][0][:], xres[tok, :], writes=[xts[j][1]], semowner=xts[j][1])
                        transposes_bf16(tps, otl[j][0], otl[j][1], DC, oT[j][0], oT[j][1])
                        def ev(c0, cw, pp, bp, r=r, j=j):
                            tmp, b_tmp = tmps[j]; xt, bxt = xts[j]
                            tr.op('dve', lambda e: e.tensor_tensor(out=tmp[:, 0:cw], in0=pp[:, 0:cw], in1=gate[r][0][:, c0:c0 + cw], op=ALU.mult), reads=[bp, gate[r][1], b_tmp], writes=[b_tmp])
                            tr.op('pool', lambda e: e.tensor_tensor(out=xt[:, c0:c0 + cw], in0=xt[:, c0:c0 + cw], in1=tmp[:, 0:cw], op=ALU.add), reads=[b_tmp, bxt], writes=[bxt])
                        acts.append((oT[j][0], oT[j][1], ev))
                    lin_multi(lin, acts, DC, wout_b, D)
                    for j, t in enumerate(tl[t0:t0 + NP]):
                        tr.dma(Q(), xres[t * 128:(t + 1) * 128, :], xts[j][0][:], reads=[xts[j][1]], semowner=xts[j][1])

        def peer(l, need_ctx):
            tiles = list(range(NT)) if need_ctx else list(range(NCT, NT))
            NB3 = 3
            with phase() as st:
                skT, b_skT = sbt(st, f"skT{l}", [128, 16, 128], BF16)
                for hp in range(16):
                    t_o, b_o = transpose_rows(st, psk[l, hp], 128, f"skf{l}_{hp}")
                    tr.op('dve', lambda e, hp=hp, t_o=t_o: e.tensor_copy(out=skT[:, hp, :], in_=t_o[:]), reads=[b_o, b_skT], writes=[b_skT])
                gT, b_gT = sbt(st, f"gT{l}", [128, DC, NB3 * 128], BF16)
                sc, b_sc = sbt(st, f"sc{l}", [128, NB3, 16, 128])
                TAU, b_TAU = sbt(st, f"tau{l}", [128, NB3, 8]); BI2, b_BI2 = sbt(st, f"bi2{l}", [128, NB3, 8])
                EB, b_EB = sbt(st, f"eb{l}", [128, NB3, 8])
                acc, b_acc = sbt(st, f"acc{l}", [128, NB3, D])
                for b0 in range(0, len(tiles), NB3):
                    blk = tiles[b0:b0 + NB3]
                    nb = len(blk); ntok = nb * 128
                    with phase() as s2:
                        LT = ln_temps(s2, f"lp{l}_{b0}")
                        lin = Lin(s2, DC, f"lq{l}_{b0}", 512 if DC <= 16 else 256)
                        qtoks = [sbt(s2, f"qtok{l}_{b0}_{j}", [128, 2048], BF16) for j in range(NB3)]
                        qT, b_qT = sbt(s2, f"qT{l}_{b0}", [128, 16, 128], BF16)
                        T16, b_T16 = sbt(s2, f"T16{l}_{b0}", [128, 16, 16]); tm, b_tm = sbt(s2, f"tm{l}_{b0}", [128, 256]); cd, b_cd = sbt(s2, f"cd{l}_{b0}", [128, 256])
                        B16, b_B16 = sbt(s2, f"B16{l}_{b0}", [128, 8, 16]); MX, b_MX = sbt(s2, f"MX{l}_{b0}", [128, 8]); ZS, b_ZS = sbt(s2, f"ZS{l}_{b0}", [128, 8])
                        jk, b_jk = sbt(s2, f"jkz{l}_{b0}", [128, 16])
                        acts = []
                        for i, t in enumerate(blk):
                            gv = gT[:, :, i * 128:(i + 1) * 128]
                            ln_mod_T(LT, t, K2['A2'], K2['bA2'], K2['B2'], K2['bB2'], gv, b_gT)
                            acts.append((gv, b_gT, (lambda c0, cw, pp, bp, i=i: evac_copy(qtoks[i][0][:, c0:c0 + cw], qtoks[i][1], pp[:, 0:cw], bp))))
                        lin_multi(lin, acts, DC, wq_b, 2048)
                        for i, t in enumerate(blk):
                            qtok, b_qtok = qtoks[i]
                            transposes_bf16(LT['pr'], qtok, b_qtok, 16, qT, b_qT)
                            for h4 in range(0, 16, 4):
                                pp, bp = MMP[(h4 // 4) % 2]
                                def mm(e, pp=pp, h4=h4):
                                    for j in range(4):
                                        ins = e.matmul(pp[:, j * 128:(j + 1) * 128], qT[:, h4 + j, :], skT[:, h4 + j, :], start=True, stop=True)
                                    return ins
                                tr.op('pe', mm, reads=[b_qT, b_skT], writes=[bp])
                                evac_copy(sc[:, i, h4:h4 + 4, :], b_sc, pp[:, :].rearrange("p (a b) -> p a b", b=128), bp)
                            for hp in range(16):
                                tr.op('dve', lambda e, hp=hp: e.max(out=T16[:, hp, 0:8], in_=sc[:, i, hp, :]), reads=[b_sc, b_T16], writes=[b_T16])
                                tr.op('dve', lambda e, hp=hp: e.match_replace(out=tm[:, 0:128], in_to_replace=T16[:, hp, 0:8], in_values=sc[:, i, hp, :], imm_value=-1e30), reads=[b_sc, b_T16], writes=[b_tm])
                                tr.op('dve', lambda e, hp=hp: e.max(out=T16[:, hp, 8:16], in_=tm[:, 0:128]), reads=[b_tm, b_T16], writes=[b_T16])
                            for h in range(8):
                                cd3 = cd[:, :].rearrange("p (a b) -> p a b", b=16)
                                tr.op('dve', lambda e, h=h: e.tensor_tensor(out=cd3, in0=T16[:, 2 * h, :].unsqueeze(2).broadcast_to([128, 16, 16]),
                                                                       in1=T16[:, 2 * h + 1, :].unsqueeze(1).broadcast_to([128, 16, 16]), op=ALU.add), reads=[b_T16], writes=[b_cd])
                                tr.op('dve', lambda e, h=h: e.max(out=B16[:, h, 0:8], in_=cd[:, :]), reads=[b_cd, b_B16], writes=[b_B16])
                                tr.op('dve', lambda e, h=h: e.match_replace(out=tm[:, :], in_to_replace=B16[:, h, 0:8], in_values=cd[:, :], imm_value=-1e30), reads=[b_cd, b_B16], writes=[b_tm])
                                tr.op('dve', lambda e, h=h: e.max(out=B16[:, h, 8:16], in_=tm[:, :]), reads=[b_tm, b_B16], writes=[b_B16])
                            tr.op('dve', lambda e: e.tensor_reduce(out=TAU[:, i, :], in_=B16[:, :, 8:16], axis=AX.X, op=ALU.min), reads=[b_B16, b_TAU], writes=[b_TAU])
                            tr.op('dve', lambda e: e.tensor_reduce(out=MX[:, :], in_=B16[:, :, 0:8], axis=AX.X, op=ALU.max), reads=[b_B16], writes=[b_MX])
                            tr.op('dve', lambda e: e.tensor_scalar(out=MX[:, :], in0=MX[:, :], scalar1=-1.0, scalar2=None, op0=ALU.mult), reads=[b_MX], writes=[b_MX])
                            for h in range(8):
                                tr.op('act', lambda e, h=h: e.activation(out=jk[:, :], in_=B16[:, h, :], func=AF.Exp, bias=MX[:, h:h + 1], accum_out=ZS[:, h:h + 1]),
                                      reads=[b_B16, b_MX, b_jk, b_ZS], writes=[b_jk, b_ZS])
                            tr.op('act', lambda e: e.activation(out=ZS[:, :], in_=ZS[:, :], func=AF.Ln), reads=[b_ZS], writes=[b_ZS])
                            tr.op('dve', lambda e: e.tensor_tensor(out=BI2[:, i, :], in0=TAU[:, i, :], in1=MX[:, :], op=ALU.add), reads=[b_TAU, b_MX, b_BI2], writes=[b_BI2])
                            tr.op('dve', lambda e: e.tensor_tensor(out=BI2[:, i, :], in0=BI2[:, i, :], in1=ZS[:, :], op=ALU.subtract), reads=[b_ZS, b_BI2], writes=[b_BI2])
                            tr.op('act', lambda e: e.activation(out=EB[:, i, :], in_=BI2[:, i, :], func=AF.Exp), reads=[b_BI2, b_EB], writes=[b_EB])
                    with phase() as s2:
                        putr = Ring(tr, s2, nc, f"put{l}_{b0}", 3, [128, DC, 128], BF16)
                        pvr = Ring(tr, s2, nc, f"pv{l}_{b0}", 2, [128, 16, 256], BF16)
                        WTr = Ring(tr, s2, nc, f"WT{l}_{b0}", 2, [128, 16, NB3 * 128], BF16)
                        aTr = Ring(tr, s2, nc, f"aT{l}_{b0}", 2, [128, NB3 * 128], BF16)
                        a8r = Ring(tr, s2, nc, f"a8{l}_{b0}", 2, [128, NB3, 8], F32)
                        ddr = Ring(tr, s2, nc, f"dd{l}_{b0}", 2, [128, NB3, 8, 128], BF16)
                        eer = Ring(tr, s2, nc, f"ee{l}_{b0}", 2, [128, NB3, 8, 128], BF16)
                        DG, b_DG = sbt(s2, f"DG{l}_{b0}", [128, NB3, 8, 128], BF16)
                        for i in range(nb):
                            for h in range(8):
                                tr.op('pool', lambda e, i=i, h=h: e.tensor_scalar(out=DG[:, i, h, :], in0=idb[:], scalar1=EB[:, i, h:h + 1], scalar2=None, op0=ALU.mult),
                                      reads=[b_idb, b_EB, b_DG], writes=[b_DG])
                        GPS = [BK[3], pst(s2, f"gps{l}_{b0}", [128, 512])]
                        VPS = [pst(s2, f"vps{l}_{b0}_{i}", [128, 512]) for i in range(2)]
                        vi = 0
                        for eb in range(8):
                            WT, b_WT = WTr.next()
                            for ec in range(16):
                                c = eb * 16 + ec
                                pu_t, b_pu = putr.next()
                                tr.dma(Q(), pu_t, put_b[c].rearrange("p (k e) -> p k e", e=128), writes=[b_pu], semowner=b_pu)
                                pp, bp = MMP[c % 2]
                                def mm(e, pp=pp, pu_t=pu_t):
                                    for k in range(DC):
                                        ins = e.matmul(pp[:, 0:ntok], pu_t[:, k, :], gT[:, k, 0:ntok], start=(k == 0), stop=(k == DC - 1))
                                    return ins
                                tr.op('pe', mm, reads=[b_pu, b_gT], writes=[bp])
                                aT, b_aT = aTr.next()
                                tr.op('act', lambda e, pp=pp, aT=aT: e.activation(out=aT[:, 0:ntok], in_=pp[:, 0:ntok], func=AF.Gelu), reads=[bp], writes=[b_aT])
                                gp, bgp = GPS[c % 2]
                                scv = sc[:, 0:nb, :, :].rearrange("p i (h two) n -> p i h two n", two=2)
                                s1c = scv[:, :, :, 0, c]
                                s2v = scv[:, :, :, 1, :]
                                a8, b_a8 = a8r.next(); dd, b_dd = ddr.next(); ee, b_ee = eer.next(); Gh, b_Gh = ee, b_ee
                                tr.op('dve', lambda e, s1c=s1c, a8=a8: e.tensor_tensor(out=a8[:, 0:nb, :], in0=s1c, in1=TAU[:, 0:nb, :], op=ALU.subtract), reads=[b_sc, b_TAU], writes=[b_a8])
                                tr.op('dve', lambda e, s2v=s2v, a8=a8, dd=dd: e.tensor_tensor(out=dd[:, 0:nb], in0=s2v, in1=a8[:, 0:nb, :].unsqueeze(3).broadcast_to([128, nb, 8, 128]), op=ALU.add),
                                      reads=[b_sc, b_a8], writes=[b_dd])
                                tr.op('act', lambda e, dd=dd, ee=ee: e.activation(out=ee[:, 0:nb], in_=dd[:, 0:nb], func=AF.Exp), reads=[b_dd], writes=[b_ee])
                                tr.op('dve', lambda e, dd=dd, ee=ee, Gh=Gh: e.scalar_tensor_tensor(out=Gh[:, 0:nb], in0=dd[:, 0:nb], scalar=0.0, in1=ee[:, 0:nb], op0=ALU.is_ge, op1=ALU.mult),
                                      reads=[b_dd, b_ee], writes=[b_Gh])
                                def gm(e, gp=gp, Gh=Gh):
                                    for i in range(nb):
                                        for h in range(8):
                                            ins = e.matmul(gp[:, i * 128:(i + 1) * 128], Gh[:, i, h, :], DG[:, i, h, :], start=(h == 0), stop=(h == 7))
                                    return ins
                                tr.op('pe', gm, reads=[b_Gh, b_DG], writes=[bgp])
                                tr.op('dve', lambda e, gp=gp, WT=WT, ec=ec, aT=aT: e.tensor_tensor(out=WT[:, ec, 0:ntok], in0=gp[:, 0:ntok], in1=aT[:, 0:ntok], op=ALU.mult), reads=[bgp, b_aT, b_WT], writes=[b_WT])
                            for c0, cw in pieces(D, 256):
                                pv_t, b_pv = pvr.next()
                                tr.dma(Q(), pv_t[:, :, 0:cw], pv_b[eb * 2048:(eb + 1) * 2048, c0:c0 + cw].rearrange("(c p) d -> p c d", p=128), writes=[b_pv], semowner=b_pv)
                                for i in range(nb):
                                    vp, bvp = VPS[vi % 2]; vi += 1
                                    def vm(e, vp=vp, pv_t=pv_t, i=i, cw=cw, WT=WT):
                                        for ec in range(16):
                                            ins = e.matmul(vp[:, 0:cw], WT[:, ec, i * 128:(i + 1) * 128], pv_t[:, ec, 0:cw], start=(ec == 0), stop=(ec == 15))
                                        return ins
                                    tr.op('pe', vm, reads=[b_WT, b_pv], writes=[bvp])
                                    if eb == 0:
                                        evac_copy(acc[:, i, c0:c0 + cw], b_acc, vp[:, 0:cw], bvp)
                                    else:
                                        tr.op('dve', lambda e, vp=vp, i=i, c0=c0, cw=cw: e.tensor_tensor(out=acc[:, i, c0:c0 + cw], in0=acc[:, i, c0:c0 + cw], in1=vp[:, 0:cw], op=ALU.add), reads=[bvp, b_acc], writes=[b_acc])
                    with phase() as s2:
                        gate = [bload(s2, f"g5_{l}_{b0}_{r}", mod_d[r, 5 * D:6 * D], D) for r in range(2 if need_ctx else 1)]
                        xr = Ring(tr, s2, nc, f"xp{l}_{b0}", 2, [128, D], F32)
                        for i, t in enumerate(blk):
                            r = 1 if t < NCT else 0
                            xt, bxt = xr.next()
                            tok = slice(t * 128, (t + 1) * 128)
                            tr.dma(Q(), xt, xres[tok, :], writes=[bxt], semowner=bxt)
                            tr.op('dve', lambda e, i=i, r=r: e.tensor_tensor(out=acc[:, i, :], in0=acc[:, i, :], in1=gate[r][0][:, :], op=ALU.mult), reads=[gate[r][1], b_acc], writes=[b_acc])
                            tr.op('pool', lambda e, i=i, xt=xt: e.tensor_tensor(out=xt, in0=xt, in1=acc[:, i, :], op=ALU.add), reads=[b_acc, bxt], writes=[bxt])
                            tr.dma(Q(), xres[tok, :], xt, reads=[bxt], semowner=bxt)

        for l in range(L):
            need_ctx = l < L - 1
            with phase() as st:
                cast_copy(st, w_in[l], win_b, D, P_IN, B_w['win'], f"cw{l}")
            with phase() as st:
                cast_copy(st, wuq[l], wuq_b, 1536, QW, B_w['wuq'], f"cwa{l}")
                cast_copy(st, wukv[l], wukv_b, 512, KVW, B_w['wukv'], f"cwb{l}")
            with phase() as st:
                cast_copy(st, w_out[l], wout_b, D, D, B_w['wout'], f"cwc{l}")
                cast_copy(st, pwq[l], wq_b, D, 2048, B_w['wq'], f"cwd{l}")
            stop = debug[1] if (debug and debug[0] == 'stop') else None
            order = ['tables', 'mod', 'mixer_prep', 'attention', 'out_proj', 'peer']
            upto = order.index(stop) if stop else len(order) - 1
            if (debug and debug[0] == 'dram' and debug[2:] and debug[2] == ('stop_mid', l)):
                upto = order.index('out_proj')
            if upto >= 0 and not (stop and stop.startswith('no_tables')):
                prep_tables(l)
            if upto >= 1:
                phase_mod(l)
            if upto >= 2:
                with phase() as st:
                    K2 = load_layer_consts(st, l)
                    mixer_prep(l, K2, need_ctx)
                    if upto >= 3:
                        attention(l, need_ctx)
                    if upto >= 4:
                        out_proj(l, need_ctx)
                    if upto >= 5:
                        peer(l, need_ctx)

        if dumps:
            with phase() as st:
                for di, (v2, od) in enumerate(dumps):
                    ring = Ring(tr, st, nc, f"dmp{di}", 2, [128, v2.shape[1]], v2.dtype)
                    for r0 in range(0, v2.shape[0], 128):
                        r1 = min(r0 + 128, v2.shape[0])
                        ap, b = ring.next()
                        tr.dma('sp', ap[0:r1 - r0, :], v2[r0:r1, :], writes=[b], semowner=b)
                        tr.dma('sp', od[r0:r1, :], ap[0:r1 - r0, :], reads=[b], semowner=b)
        with phase() as st:
            ring = Ring(tr, st, nc, "ocp", 3, [128, D], F32)
            toks = []
            for t in range(NCT, NT):
                ap, b = ring.next()
                q = Q()
                tr.dma(q, ap, xres[t * 128:(t + 1) * 128, :], reads=[B_xres[t]], writes=[b], semowner=b)
                toks.append((q, tr.dma(q, out[(t - NCT) * 128:(t - NCT + 1) * 128, :], ap, reads=[b], writes=[B_out], semowner=b)))
            for q, tk in toks[-6:]:
                tr.wait_all(q, [tk])
                tr.wait_all('sp', [tk])
    return nc


def rope_tables(SEQ, rot, grid_w=64):
    rows = SEQ // grid_w
    row = np.repeat(np.arange(rows, dtype=np.float32), grid_w)
    col = np.tile(np.arange(grid_w, dtype=np.float32), rows)
    nf = rot // 4
    inv = (10000.0 ** (-np.arange(nf, dtype=np.float32) / nf)).astype(np.float32)
    ar = row[:, None] * inv
    ac = col[:, None] * inv
    ang = np.concatenate([ar, ar, ac, ac], axis=-1)
    cos, sin = np.cos(ang).astype(np.float32), np.sin(ang).astype(np.float32)
    sgn = np.tile(np.concatenate([-np.ones(nf), np.ones(nf)]), 2).astype(np.float32)
    return np.stack([cos, sin * sgn], axis=1)


def make_in_maps(cfg, inputs):
    D, SEQ, CTX, L, DC = cfg['D'], cfg['SEQ'], cfg['CTX'], cfg['DEPTH'], cfg['DC']
    f = lambda a: np.ascontiguousarray(np.asarray(a, dtype=np.float32))
    shared = {k: f(inputs[k]) for k in ('w_mod', 'b_mod', 'w_in', 'a_v_gain', 'a_w_s', 'a_b_s', 'b_q_gain', 'b_kv_gain',
                                        'b_w_uq', 'b_w_ukv', 'b_qn_gain', 'b_kn_gain', 'c_qn_gain', 'c_kn_gain', 'c_sink',
                                        'w_out', 'peer_w_q', 'peer_u', 'peer_v')}
    shared['norm1_gain'] = f(inputs['norm1_gain']).reshape(L, DC, 128)
    shared['norm2_gain'] = f(inputs['norm2_gain']).reshape(L, DC, 128)
    shared['peer_subkeys'] = f(inputs['peer_subkeys']).reshape(L, 16, 128, 128)
    shared['c_ctx'] = f(inputs['c_ctx']).reshape(DC, 128)
    shared['ropeB'] = rope_tables(SEQ, 64)
    shared['ropeC'] = rope_tables(SEQ, 128)
    j = np.arange(128)[:, None]; i = np.arange(128)[None, :]
    shared['masks'] = np.stack([(j >= i), (j <= i)]).astype(np.float32)
    maps = []
    for b in range(cfg['BATCH']):
        m = dict(shared)
        m['x'] = f(inputs['x'][b]); m['ctx'] = f(inputs['ctx'][b]); m['c'] = f(inputs['c'][b]).reshape(DC, 128)
        maps.append(m)
    return maps


def kernel(**inputs):
    cfg = make_cfg()
    nc = build(cfg)
    maps = make_in_maps(cfg, inputs)
    res = run_bass_kernel_spmd(nc, maps, core_ids=list(range(cfg['BATCH'])))
    return np.stack([r['out'] for r in res.results], axis=0).astype(np.float32)
```

```python
import contextlib
import numpy as np
import ml_dtypes
import concourse.bass as bass
import concourse.mybir as mybir
from concourse.bass_utils import run_bass_kernel_spmd

F32 = mybir.dt.float32
BF16 = mybir.dt.bfloat16
ALU = mybir.AluOpType
AF = mybir.ActivationFunctionType
AX = mybir.AxisListType
EPS = 1e-6


def make_cfg(D=4096, SEQ=2048, CTX=256, DEPTH=2, BATCH=4):
    c = dict(D=D, SEQ=SEQ, CTX=CTX, DEPTH=DEPTH, BATCH=BATCH)
    NS = D // 128
    c['A_H'] = NS // 4
    c['B_H'] = (NS - c['A_H']) // 2
    c['C_H'] = NS - c['A_H'] - c['B_H']
    c['C_KV'] = c['C_H'] // 3
    c['A_W'] = c['A_H'] * 128
    c['Q_LORA'] = 1536
    c['KV_LORA'] = 512
    c['P_A'] = 2 * c['A_W']
    c['P_B'] = 1536 + 512 + 64
    c['P_C'] = (c['C_H'] + 2 * c['C_KV']) * 128
    c['P_IN'] = c['P_A'] + c['P_B'] + c['P_C']
    c['DC'] = D // 128
    c['NT'] = (CTX + SEQ) // 128
    c['NCT'] = CTX // 128
    return c


class Buf:
    def __init__(self, name):
        self.name = name
        self.last_w = None
        self.readers = []
        self.dsem = None
        self.dcount = 0


class Eng:
    def __init__(self, name, eng, sem):
        self.name, self.eng, self.sem = name, eng, sem
        self.count = 0
        self.seen = {}


class Tracker:
    def __init__(self, nc, stack):
        self.nc = nc
        self.stack = stack
        self.engs = {}
        for name, e in (('pe', nc.tensor), ('dve', nc.vector), ('act', nc.scalar), ('pool', nc.gpsimd),
                        ('sp', nc.sync)):
            sem = stack.enter_context(nc.semaphore('prog_' + name))
            self.engs[name] = Eng(name, e, sem)
        self.nbuf = 0
        self.dma_toks = {}
        self.free_dsems = []
        self.phase_bufs = [[]]
        self.ndsem = 0

    def push_phase(self):
        self.phase_bufs.append([])

    def pop_phase(self):
        for b in self.phase_bufs.pop():
            if b.dsem is not None:
                self.free_dsems.append((b.dsem, b.dcount, b.dkey))
                b.dsem = None

    def barrier(self):
        toks = [(E.name, E.sem, E.count) for E in self.engs.values() if E.count > 0] + list(self.dma_toks.values())
        for E in self.engs.values():
            for t in toks:
                if t[0] != E.name:
                    self._wait(E, t)

    def buf(self, name=None):
        self.nbuf += 1
        b = Buf(name or f'b{self.nbuf}')
        self.phase_bufs[-1].append(b)
        return b

    def _wait(self, E, tok):
        if tok is None:
            return
        key, sem, cnt = tok
        if key == E.name and E.name in ('pe',):
            return
        if E.seen.get(key, 0) >= cnt:
            return
        E.eng.wait_ge(sem, cnt)
        E.seen[key] = cnt

    def need(self, E, reads=(), writes=()):
        E = self.engs[E] if isinstance(E, str) else E
        for b in reads:
            self._wait(E, b.last_w)
        for b in writes:
            self._wait(E, b.last_w)
            for r in b.readers:
                self._wait(E, r)

    def done(self, E, ins, reads=(), writes=()):
        E = self.engs[E] if isinstance(E, str) else E
        E.count += 1
        ins.then_inc(E.sem, 1)
        tok = (E.name, E.sem, E.count)
        for b in reads:
            b.readers.append(tok)
            if len(b.readers) > 24:
                b.readers = b.readers[-24:]
        for b in writes:
            b.last_w = tok
            b.readers = []
        return tok

    def op(self, E, fn, reads=(), writes=()):
        Eo = self.engs[E]
        self.need(Eo, reads, writes)
        ins = fn(Eo.eng)
        return self.done(Eo, ins, reads, writes)

    def dma(self, Q, out, in_, reads=(), writes=(), semowner=None, **kw):
        Eo = self.engs[Q]
        self.need(Eo, reads, writes)
        ow = semowner
        if ow.dsem is None:
            if self.free_dsems:
                ow.dsem, ow.dcount, ow.dkey = self.free_dsems.pop()
            else:
                self.ndsem += 1
                ow.dkey = f'd{self.ndsem}'
                ow.dsem = self.stack.enter_context(self.nc.semaphore(ow.dkey))
                ow.dcount = 0
        ins = Eo.eng.dma_start(out=out, in_=in_, **kw)
        ow.dcount += 16
        ins.then_inc(ow.dsem, 16)
        tok = (ow.dkey, ow.dsem, ow.dcount)
        self.dma_toks[tok[0]] = tok
        for b in reads:
            b.readers.append(tok)
        for b in writes:
            b.last_w = tok
            b.readers = []
        return tok

    def wait_all(self, E, toks):
        Eo = self.engs[E]
        for t in toks:
            self._wait(Eo, t)


class Ring:
    def __init__(self, tr, stack, nc, name, n, shape, dtype):
        self.n = n
        self.t = stack.enter_context(nc.sbuf_tensor(name, [shape[0], n] + list(shape[1:]), dtype))
        self.bufs = [tr.buf(f'{name}{i}') for i in range(n)]
        self.i = 0

    def next(self):
        k = self.i % self.n
        self.i += 1
        return self.t[:, k], self.bufs[k]


def pieces(n, step=512):
    return [(i, min(step, n - i)) for i in range(0, n, step)]


def build(cfg, debug=None):
    D, SEQ, CTX, DEPTH = cfg['D'], cfg['SEQ'], cfg['CTX'], cfg['DEPTH']
    DC, NT, NCT = cfg['DC'], cfg['NT'], cfg['NCT']
    A_H, B_H, C_H, C_KV, A_W = cfg['A_H'], cfg['B_H'], cfg['C_H'], cfg['C_KV'], cfg['A_W']
    P_IN = cfg['P_IN']
    NTOK = CTX + SEQ
    L = DEPTH
    QW, KVW = B_H * 192, B_H * 256
    oB, oC = 2 * A_W, 2 * A_W + 2112
    nc = bass.Bass("TRN2", target_bir_lowering=False)

    def din(name, shape):
        return nc.dram_tensor(name, list(shape), F32, kind="ExternalInput")
    x_in = din("x", [SEQ, D]); ctx_in = din("ctx", [CTX, D]); c_in = din("c", [DC, 128]); cc_in = din("c_ctx", [DC, 128])
    w_mod = din("w_mod", [L, D, 6 * D]); b_mod = din("b_mod", [L, 6 * D])
    g1 = din("norm1_gain", [L, DC, 128]); g2 = din("norm2_gain", [L, DC, 128])
    w_in = din("w_in", [L, D, P_IN]); a_vg = din("a_v_gain", [L, A_W]); a_ws = din("a_w_s", [L, 128, 128]); a_bs = din("a_b_s", [L, 128])
    bqg = din("b_q_gain", [L, 1536]); bkvg = din("b_kv_gain", [L, 512]); wuq = din("b_w_uq", [L, 1536, QW]); wukv = din("b_w_ukv", [L, 512, KVW])
    bqn = din("b_qn_gain", [L, 192]); bkn = din("b_kn_gain", [L, 192]); cqn = din("c_qn_gain", [L, 128]); ckn = din("c_kn_gain", [L, 128])
    csink = din("c_sink", [L, C_H]); w_out = din("w_out", [L, D, D]); pwq = din("peer_w_q", [L, D, 2048])
    psk = din("peer_subkeys", [L, 16, 128, 128]); pu = din("peer_u", [L, 16384, D]); pvt = din("peer_v", [L, 16384, D])
    ropeB = din("ropeB", [SEQ, 2, 64]); ropeC = din("ropeC", [SEQ, 2, 128])
    masks = din("masks", [2, 128, 128])
    out = nc.dram_tensor("out", [SEQ, D], F32, kind="ExternalOutput")

    def dscr(name, shape, dt=BF16):
        return nc.dram_tensor(name, list(shape), dt)
    xres = dscr("xres", [NTOK, D], F32)
    win_b = dscr("win_b", [D, P_IN]); wout_b = dscr("wout_b", [D, D]); wq_b = dscr("wq_b", [D, 2048])
    wuq_b = dscr("wuq_b", [1536, QW]); wukv_b = dscr("wukv_b", [512, KVW])
    put_b = dscr("put_b", [128, 128, D])
    pv_b = dscr("pv_b", [16384, D])
    mod_d = dscr("mod_d", [2, 6 * D], F32)
    qBT = dscr("qBT", [B_H, 192, NTOK]); kBT = dscr("kBT", [B_H, 192, NTOK]); vB = dscr("vB", [NTOK, B_H * 128])
    qCT = dscr("qCT", [C_H, 128, NTOK]); kCT = dscr("kCT", [C_KV, 128, NTOK]); vC = dscr("vC", [NTOK, C_KV * 128])
    o_d = dscr("o_d", [NTOK, D])
    dbg = nc.dram_tensor("dbg", list(debug[1]), F32, kind="ExternalOutput") if (debug and debug[0] == 'ptile') else None
    SCR = dict(qBT=qBT, kBT=kBT, vB=vB, qCT=qCT, kCT=kCT, vC=vC, o_d=o_d, xres=xres, mod_d=mod_d)
    dumps = []
    if debug and debug[0] == 'dram':
        for nm in debug[1]:
            tns = SCR[nm]
            v2 = tns.ap() if len(tns.shape) == 2 else tns.ap().rearrange("h w t -> (h w) t")
            od = nc.dram_tensor("dbg_" + nm, list(v2.shape), tns.dtype, kind="ExternalOutput")
            dumps.append((v2, od))

    with contextlib.ExitStack() as top:
        tr = Tracker(nc, top)
        top.enter_context(nc.Block())
        dq = ['sp', 'pool']
        dqi = [0]

        def Q():
            dqi[0] += 1
            return dq[dqi[0] % 2]
        B_xres = [tr.buf(f'xres{t}') for t in range(NT)]
        B_w = {n: tr.buf('W' + n) for n in ('win', 'wout', 'wq', 'wuq', 'wukv', 'put', 'pv')}
        B_mod = tr.buf('modd')
        B_qkv = tr.buf('qkv')
        B_o = [tr.buf(f'o{t}') for t in range(NT)]
        B_out = tr.buf('out')

        @contextlib.contextmanager
        def phase():
            tr.push_phase()
            with contextlib.ExitStack() as st_:
                yield st_
                tr.barrier()
            tr.pop_phase()

        def sbt(st, name, shape, dt=F32):
            return st.enter_context(nc.sbuf_tensor(name, list(shape), dt)), tr.buf(name)

        def pst(st, name, shape, dt=F32):
            return st.enter_context(nc.psum_tensor(name, list(shape), dt)), tr.buf(name)

        idf, b_idf = sbt(top, "idf", [128, 128]); idb, b_idb = sbt(top, "idb", [128, 128], BF16)
        tr.op('pool', lambda e: e.memset(idf[:], 0.0), writes=[b_idf])
        tr.op('pool', lambda e: e.affine_select(out=idf[:], in_=idf[:], pattern=[[-1, 128]], compare_op=ALU.not_equal,
                                                fill=1.0, base=0, channel_multiplier=1), reads=[b_idf], writes=[b_idf])
        tr.op('dve', lambda e: e.tensor_copy(out=idb[:], in_=idf[:]), reads=[b_idf], writes=[b_idb])
        mk, b_mk = sbt(top, "mk", [128, 2, 128], BF16)
        with phase() as st:
            mf, b_mf = sbt(st, "mf", [128, 2, 128])
            tr.dma('sp', mf[:], masks.ap().rearrange("m p j -> p m j"), writes=[b_mf], semowner=b_mf)
            tr.op('dve', lambda e: e.tensor_copy(out=mk[:], in_=mf[:]), reads=[b_mf], writes=[b_mk])

        BK = []
        for i in range(4):
            BK.append(pst(top, f"bank{i}", [128, 512]))
        tr_in, b_trin = sbt(top, "tr_in", [128, 128]); tr_ps, b_trps = BK[0][0][:, 0:128], BK[0][1]
        MMP = [BK[1], BK[2]]

        def transpose_rows(st, src2d, nrows, name):
            t_o, b_o = sbt(st, name, [128, nrows])
            tr.dma('sp', tr_in[0:nrows, :], src2d, writes=[b_trin], semowner=b_trin)
            tr.op('pe', lambda e: e.transpose(tr_ps[:, 0:nrows], tr_in[0:nrows, :], idf[0:nrows, 0:nrows]), reads=[b_trin, b_idf], writes=[b_trps])
            tr.op('dve', lambda e: e.tensor_copy(out=t_o[:], in_=tr_ps[:, 0:nrows]), reads=[b_trps], writes=[b_o])
            return t_o, b_o

        with phase() as st:
            ring = Ring(tr, st, nc, "xcp", 3, [128, D], F32)
            for t in range(NT):
                src = ctx_in[t * 128:(t + 1) * 128, :] if t < NCT else x_in[(t - NCT) * 128:(t - NCT + 1) * 128, :]
                ap, b = ring.next()
                q = Q()
                tr.dma(q, ap, src, writes=[b], semowner=b)
                tr.dma(q, xres[t * 128:(t + 1) * 128, :], ap, reads=[b], writes=[B_xres[t]], semowner=b)

        scT, b_scT = sbt(top, "scT", [128, DC, 2])
        with phase() as st:
            for r, src in enumerate((c_in, cc_in)):
                t_o, b_o = transpose_rows(st, src[:, :], DC, f"cT{r}")
                tr.op('act', lambda e: e.activation(out=scT[:, :, r], in_=t_o[:], func=AF.Silu), reads=[b_o, b_scT], writes=[b_scT])

        cast_engs = ['dve', 'act', 'pool']
        ci = [0]

        def cast(out_ap, in_ap, reads, writes):
            ci[0] += 1
            en = cast_engs[ci[0] % 3]
            if en == 'act':
                return tr.op('act', lambda e: e.copy(out=out_ap, in_=in_ap), reads=reads, writes=writes)
            return tr.op(en, lambda e: e.tensor_copy(out=out_ap, in_=in_ap), reads=reads, writes=writes)

        def cast_copy(st, src, dst, rows, cols, bdst, tag):
            rf = Ring(tr, st, nc, tag + "f", 4, [128, 2048], F32)
            rb = Ring(tr, st, nc, tag + "b", 4, [128, 2048], BF16)
            for r0 in range(0, rows, 128):
                for c0, cw in pieces(cols, 2048):
                    fa, fb = rf.next(); ba, bb = rb.next()
                    q = Q()
                    tr.dma(q, fa[:, 0:cw], src[r0:r0 + 128, c0:c0 + cw], writes=[fb], semowner=fb)
                    cast(ba[:, 0:cw], fa[:, 0:cw], [fb], [bb])
                    tr.dma(q, dst[r0:r0 + 128, c0:c0 + cw], ba[:, 0:cw], reads=[bb], semowner=bb)

        def prep_tables(l):
            with phase() as st:
                cast_copy(st, pvt[l], pv_b, 16384, D, B_w['pv'], f"cwv{l}")
            with phase() as st:
                rf = Ring(tr, st, nc, f"uf{l}", 2, [128, D], F32)
                rb = Ring(tr, st, nc, f"ub{l}", 2, [128, DC, 128], BF16)
                pr = [pst(st, f"up{l}_{i}", [128, 4, 128]) for i in range(2)]
                k = 0
                for ec in range(128):
                    ba, bb = rb.next()
                    fa, fb = rf.next()
                    tr.dma(Q(), fa, pu[l, ec * 128:(ec + 1) * 128, :], writes=[fb], semowner=fb)
                    for c4 in range(0, DC, 4):
                        pp, bp = pr[k % 2]; k += 1
                        def tp(e, pp=pp, fa=fa, c4=c4):
                            for i in range(4):
                                ins = e.transpose(pp[:, i, :], fa[:, (c4 + i) * 128:(c4 + i + 1) * 128], idf[:])
                            return ins
                        tr.op('pe', tp, reads=[fb, b_idf], writes=[bp])
                        evac_copy(ba[:, c4:c4 + 4, :], bb, pp[:, :, :], bp)
                    tr.dma(Q(), put_b[ec].rearrange("p (k e) -> p k e", e=128), ba, reads=[bb], semowner=bb)

        def rstd_from_ss(ss_ap, b_ss, n):
            tr.op('dve', lambda e: e.tensor_scalar(out=ss_ap, in0=ss_ap, scalar1=1.0 / n, scalar2=EPS, op0=ALU.mult, op1=ALU.add), reads=[b_ss], writes=[b_ss])
            tr.op('act', lambda e: e.activation(out=ss_ap, in_=ss_ap, func=AF.Sqrt), reads=[b_ss], writes=[b_ss])
            tr.op('dve', lambda e: e.reciprocal(out=ss_ap, in_=ss_ap), reads=[b_ss], writes=[b_ss])

        def bload(st, name, src1d, n):
            t, b = sbt(st, name, [128, n])
            tr.dma('sp', t[:], src1d.partition_broadcast(128), writes=[b], semowner=b)
            return t, b

        def phase_mod(l):
            with phase() as st:
                wr = Ring(tr, st, nc, f"wm{l}", 2, [128, DC, 512], F32)
                pm = [(MMP[i][0][0:2, :], MMP[i][1]) for i in range(2)]
                br = Ring(tr, st, nc, f"bm{l}", 2, [2, 512], F32)
                mr = Ring(tr, st, nc, f"mr{l}", 2, [2, 512], F32)
                for i, (c0, cw) in enumerate(pieces(6 * D)):
                    wt, bw = wr.next()
                    tr.dma(Q(), wt, w_mod[l, :, c0:c0 + cw].rearrange("(k p) c -> p k c", p=128), writes=[bw], semowner=bw)
                    bt, bb = br.next()
                    tr.dma('sp', bt, b_mod[l, c0:c0 + cw].partition_broadcast(2), writes=[bb], semowner=bb)
                    pp, bp = pm[i % 2]
                    def mm(e, pp=pp, wt=wt):
                        for k in range(DC):
                            ins = e.matmul(pp[:, :], scT[:, k, :], wt[:, k, :], start=(k == 0), stop=(k == DC - 1))
                        return ins
                    tr.op('pe', mm, reads=[bw, b_scT], writes=[bp])
                    mt, bmt = mr.next()
                    tr.op('dve', lambda e, mt=mt, pp=pp, bt=bt: e.tensor_tensor(out=mt, in0=pp[:, :], in1=bt, op=ALU.add), reads=[bp, bb], writes=[bmt])
                    tr.dma('sp', mod_d[:, c0:c0 + cw], mt, reads=[bmt], semowner=bmt)

        def load_layer_consts(st, l):
            K = {}
            for nm in ("1", "2"):
                K['A' + nm], K['bA' + nm] = sbt(st, f"A{nm}_{l}", [128, DC, 2])
                K['B' + nm], K['bB' + nm] = sbt(st, f"B{nm}_{l}", [128, DC, 2])
            with phase() as s2:
                mT = {}
                for i in (0, 1, 3, 4):
                    for r in range(2):
                        mT[i, r] = transpose_rows(s2, mod_d[r, i * D:(i + 1) * D].rearrange("(c p) -> c p", p=128), DC, f"mT{l}_{i}{r}")
                g1T = transpose_rows(s2, g1[l], DC, f"g1T{l}")
                g2T = transpose_rows(s2, g2[l], DC, f"g2T{l}")
                for nm, gT, isc, ish in (("1", g1T, 1, 0), ("2", g2T, 4, 3)):
                    A, bA, Bt, bB = K['A' + nm], K['bA' + nm], K['B' + nm], K['bB' + nm]
                    for r in range(2):
                        tr.op('dve', lambda e, A=A, r=r, gT=gT, isc=isc: e.scalar_tensor_tensor(out=A[:, :, r], in0=mT[isc, r][0][:], scalar=1.0, in1=gT[0][:], op0=ALU.add, op1=ALU.mult),
                              reads=[mT[isc, r][1], gT[1], bA], writes=[bA])
                        tr.op('dve', lambda e, Bt=Bt, r=r, ish=ish: e.tensor_copy(out=Bt[:, :, r], in_=mT[ish, r][0][:]), reads=[mT[ish, r][1], bB], writes=[bB])
            return K

        def ln_temps(st, tag):
            T = {}
            T['xt'], T['bxt'] = sbt(st, tag + "x", [128, D])
            T['jk'], T['bjk'] = sbt(st, tag + "j", [128, D], BF16)
            T['ss'], T['bss'] = sbt(st, tag + "s", [128, 1])
            T['pr'] = [pst(st, f"{tag}p{i}", [128, 4, 128], BF16) for i in range(2)]
            return T

        def ln_mod_T(T, t, A, bA, Bm, bB, hT, b_hT):
            r = 1 if t < NCT else 0
            xt, bxt, jk, bjk, ss, bss, pr = T['xt'], T['bxt'], T['jk'], T['bjk'], T['ss'], T['bss'], T['pr']
            tr.dma(Q(), xt[:], xres[t * 128:(t + 1) * 128, :], reads=[B_xres[t]], writes=[bxt], semowner=bxt)
            tr.op('act', lambda e: e.activation(out=jk[:], in_=xt[:], func=AF.Square, accum_out=ss[:]), reads=[bxt], writes=[bjk, bss])
            rstd_from_ss(ss[:], bss, D)
            tr.op('dve', lambda e: e.tensor_scalar(out=jk[:], in0=xt[:], scalar1=ss[:, 0:1], scalar2=None, op0=ALU.mult), reads=[bxt, bss], writes=[bjk])
            for i, c4 in enumerate(range(0, DC, 4)):
                pp, bp = pr[i % 2]
                def tp(e, pp=pp, c4=c4):
                    for j in range(4):
                        ins = e.transpose(pp[:, j, :], jk[:, (c4 + j) * 128:(c4 + j + 1) * 128], idb[:])
                    return ins
                tr.op('pe', tp, reads=[bjk, b_idb], writes=[bp])
                for j in range(4):
                    c = c4 + j
                    en = 'dve' if j % 2 == 0 else 'pool'
                    if en == 'pool':
                        en = 'dve'
                    tr.op(en, lambda e, pp=pp, j=j, c=c: e.tensor_scalar(out=hT[:, c, :], in0=pp[:, j, :], scalar1=A[:, c, r:r + 1], scalar2=Bm[:, c, r:r + 1], op0=ALU.mult, op1=ALU.add),
                          reads=[bp, bA, bB, b_hT], writes=[b_hT])
            return xt, bxt

        class Lin:
            def __init__(self, st, KCmax, tag, pw=512, depth=2):
                self.pw = pw
                self.wr = Ring(tr, st, nc, tag + "w", depth, [128, KCmax, pw], BF16)
                self.ps = [(MMP[i][0], MMP[i][1]) for i in range(2)]
                self.i = 0

            def run(self, actT, b_act, KC, w_d, ncols, evac, col0=0):
                for c0, cw in pieces(ncols, self.pw):
                    wt, bw = self.wr.next()
                    tr.dma(Q(), wt[:, 0:KC, 0:cw], w_d[0:KC * 128, col0 + c0:col0 + c0 + cw].rearrange("(k p) c -> p k c", p=128), writes=[bw], semowner=bw)
                    pp, bp = self.ps[self.i % 2]; self.i += 1
                    def mm(e, pp=pp, wt=wt, cw=cw):
                        for k in range(KC):
                            ins = e.matmul(pp[:, 0:cw], actT[:, k, :], wt[:, k, 0:cw], start=(k == 0), stop=(k == KC - 1))
                        return ins
                    tr.op('pe', mm, reads=[bw, b_act], writes=[bp])
                    evac(c0, cw, pp, bp)

        def lin_multi(lin, acts, KC, w_d, ncols):
            for c0, cw in pieces(ncols, lin.pw):
                wt, bw = lin.wr.next()
                tr.dma(Q(), wt[:, 0:KC, 0:cw], w_d[0:KC * 128, c0:c0 + cw].rearrange("(k p) c -> p k c", p=128), writes=[bw], semowner=bw)
                for (actT, b_act, evac) in acts:
                    pp, bp = lin.ps[lin.i % 2]; lin.i += 1
                    def mm(e, pp=pp, wt=wt, cw=cw, actT=actT):
                        for k in range(KC):
                            ins = e.matmul(pp[:, 0:cw], actT[:, k, :], wt[:, k, 0:cw], start=(k == 0), stop=(k == KC - 1))
                        return ins
                    tr.op('pe', mm, reads=[bw, b_act], writes=[bp])
                    evac(c0, cw, pp, bp)

        evi = [0]

        def evac_copy(out_ap, b_out, pp_ap, bp):
            evi[0] += 1
            if evi[0] % 2:
                tr.op('dve', lambda e: e.tensor_copy(out=out_ap, in_=pp_ap), reads=[bp, b_out], writes=[b_out])
            else:
                tr.op('act', lambda e: e.copy(out=out_ap, in_=pp_ap), reads=[bp, b_out], writes=[b_out])

        def transposes_bf16(st_ps, src, b_src, nchunks, dst, b_dst, width=128):
            for c4 in range(0, nchunks, 4):
                pp, bp = st_ps[(c4 // 4) % 2]
                n = min(4, nchunks - c4)
                def tp(e, pp=pp, c4=c4, n=n):
                    for j in range(n):
                        ins = e.transpose(pp[0:width, j, :], src[:, (c4 + j) * width:(c4 + j + 1) * width], idb[:])
                    return ins
                tr.op('pe', tp, reads=[b_src, b_idb], writes=[bp])
                evac_copy(dst[0:width, c4:c4 + n, :], b_dst, pp[0:width, 0:n, :], bp)

        def mixer_prep(l, K, need_ctx):
            with phase() as st:
                PW = 512 if DC <= 16 else 256
                hT, b_hT = sbt(st, f"hT{l}", [128, DC, 128], BF16)
                LT = ln_temps(st, f"ln{l}")
                lin = Lin(st, max(DC, 12), f"l1{l}", PW, depth=3)
                pt, bpt = sbt(st, f"ptile{l}", [128, P_IN])
                avg_b = bload(st, f"avg{l}", a_vg[l], A_W); qg_b = bload(st, f"qg{l}", bqg[l], 1536); kvg_b = bload(st, f"kvg{l}", bkvg[l], 512)
                bqn_b = bload(st, f"bqn{l}", bqn[l], 192); bkn_b = bload(st, f"bkn{l}", bkn[l], 192)
                cqn_b = bload(st, f"cqn{l}", cqn[l], 128); ckn_b = bload(st, f"ckn{l}", ckn[l], 128)
                wsT_f = transpose_rows(st, a_ws[l], 128, f"wsT{l}")
                wsT, b_wsT = sbt(st, f"wsTb{l}", [128, 128], BF16)
                tr.op('dve', lambda e: e.tensor_copy(out=wsT[:], in_=wsT_f[0][:]), reads=[wsT_f[1]], writes=[b_wsT])
                bs_c, b_bs = sbt(st, f"bsc{l}", [128, 1])
                tr.dma('sp', bs_c[:], a_bs[l].rearrange("(p o) -> p o", o=1), writes=[b_bs], semowner=b_bs)
                SW = max(B_H * 192, A_W)
                S1, b_S1 = sbt(st, f"S1{l}", [128, SW]); S2, b_S2 = sbt(st, f"S2{l}", [128, SW]); S3, b_S3 = sbt(st, f"S3{l}", [128, SW])
                ssH, b_ssH = sbt(st, f"ssH{l}", [128, 16]); ss1, b_ss1 = sbt(st, f"ss1{l}", [128, 1])
                qf, b_qf = sbt(st, f"qf{l}", [128, B_H * 192]); kvf, b_kvf = sbt(st, f"kvf{l}", [128, B_H * 256])
                NB, b_NB = sbt(st, f"NB{l}", [128, SW], BF16); hTt, b_hTt = sbt(st, f"hTt{l}", [128, 2 * B_H, 128], BF16)
                cqb, b_cqb = sbt(st, f"cqb{l}", [128, 1536], BF16); cqT, b_cqT = sbt(st, f"cqT{l}", [128, 12, 128], BF16)
                vb, b_vb = sbt(st, f"vb{l}", [128, B_H * 128], BF16); ot, b_ot = sbt(st, f"ot{l}", [128, A_W], BF16)
                tbB, b_tbB = sbt(st, f"tbB{l}", [128, 2, 64]); tbC, b_tbC = sbt(st, f"tbC{l}", [128, 2, 128])
                tps = [pst(st, f"tp{l}_{i}", [128, 4, 128], BF16) for i in range(2)]

                def norm_heads(src3, b_src, H, W, gain3, b_gain, dst3, b_dst):
                    sq = S1[:, 0:H * W].rearrange("p (h w) -> p h w", w=W)
                    tr.op('pool', lambda e: e.tensor_tensor(out=sq, in0=src3, in1=src3, op=ALU.mult), reads=[b_src], writes=[b_S1])
                    tr.op('dve', lambda e: e.tensor_reduce(out=ssH[:, 0:H], in_=sq, axis=AX.X, op=ALU.add), reads=[b_S1], writes=[b_ssH])
                    rstd_from_ss(ssH[:, 0:H], b_ssH, W)
                    tr.op('dve', lambda e: e.tensor_tensor(out=sq, in0=src3, in1=ssH[:, 0:H].unsqueeze(2).broadcast_to([128, H, W]), op=ALU.mult), reads=[b_src, b_ssH], writes=[b_S1])
                    tr.op('dve', lambda e: e.tensor_tensor(out=dst3, in0=sq, in1=gain3, op=ALU.mult), reads=[b_S1, b_gain, b_dst], writes=[b_dst])

                def rope3(x3, b_x, H, W, tab, b_tab):
                    hw = W // 4
                    t1 = S1[:, 0:H * W].rearrange("p (h w) -> p h w", w=W); t2 = S3[:, 0:H * W].rearrange("p (h w) -> p h w", w=W)
                    tr.op('dve', lambda e: e.tensor_tensor(out=t1, in0=x3, in1=tab[:, 0, :].unsqueeze(1).broadcast_to([128, H, W]), op=ALU.mult), reads=[b_x, b_tab], writes=[b_S1])
                    for a in range(2):
                        lo = (a * 2 * hw, a * 2 * hw + hw); hi = (a * 2 * hw + hw, (a + 1) * 2 * hw)
                        for (o, i) in ((lo, hi), (hi, lo)):
                            tr.op('pool', lambda e, o=o, i=i: e.tensor_tensor(out=t2[:, :, o[0]:o[1]], in0=x3[:, :, i[0]:i[1]],
                                                                             in1=tab[:, 1, o[0]:o[1]].unsqueeze(1).broadcast_to([128, H, hw]), op=ALU.mult),
                                  reads=[b_x, b_tab, b_S3], writes=[b_S3])
                    tr.op('dve', lambda e: e.tensor_tensor(out=x3, in0=t1, in1=t2, op=ALU.add), reads=[b_S1, b_S3, b_x], writes=[b_x])

                def to_T_store(H, W, dstT, tok):
                    items = []
                    for h in range(H):
                        items.append((h * W, 128, h))
                        if W == 192:
                            items.append((h * W + 128, 64, H + h))
                    for g0 in range(0, len(items), 4):
                        grp = items[g0:g0 + 4]
                        pp, bp = tps[(g0 // 4) % 2]
                        def tp(e, pp=pp, grp=grp):
                            for j, (c0, w, slot) in enumerate(grp):
                                ins = e.transpose(pp[0:w, j, :], NB[:, c0:c0 + w], idb[:])
                            return ins
                        tr.op('pe', tp, reads=[b_NB, b_idb], writes=[bp])
                        for j, (c0, w, slot) in enumerate(grp):
                            evac_copy(hTt[0:w, slot, :], b_hTt, pp[0:w, j, :], bp)
                    tr.dma(Q(), dstT[:, 0:128, tok].rearrange("h d t -> d h t"), hTt[:, 0:H, :], reads=[b_hTt], semowner=b_hTt)
                    if W == 192:
                        tr.dma(Q(), dstT[:, 128:192, tok].rearrange("h d t -> d h t"), hTt[0:64, H:2 * H, :], reads=[b_hTt], semowner=b_hTt)

                def rms_rows(src, b_src, n, gain_b, dstb, b_dst):
                    tr.op('act', lambda e: e.activation(out=dstb, in_=src, func=AF.Square, accum_out=ss1[:]), reads=[b_src, b_dst], writes=[b_dst, b_ss1])
                    rstd_from_ss(ss1[:], b_ss1, n)
                    tr.op('dve', lambda e: e.scalar_tensor_tensor(out=dstb, in0=src, scalar=ss1[:, 0:1], in1=gain_b[0][:, 0:n], op0=ALU.mult, op1=ALU.mult),
                          reads=[b_src, b_ss1, gain_b[1], b_dst], writes=[b_dst])

                for t in range(NT):
                    lat = t >= NCT
                    tok = slice(t * 128, (t + 1) * 128)
                    ln_mod_T(LT, t, K['A1'], K['bA1'], K['B1'], K['bB1'], hT, b_hT)
                    def ev_in(c0, cw, pp, bp):
                        if c0 < 2 * A_W:
                            tr.op('act', lambda e: e.activation(out=pt[:, c0:c0 + cw], in_=pp[:, 0:cw], func=AF.Gelu), reads=[bp, bpt], writes=[bpt])
                        else:
                            evac_copy(pt[:, c0:c0 + cw], bpt, pp[:, 0:cw], bp)
                    lin.run(hT, b_hT, DC, win_b, P_IN, ev_in)
                    if debug and debug[0] == 'ptile' and t == debug[2]:
                        tr.dma('sp', dbg[:, :], pt[:, :], reads=[bpt], semowner=bpt)
                    if lat:
                        p0 = (t - NCT) * 128
                        tr.dma('sp', tbB[:], ropeB[p0:p0 + 128], writes=[b_tbB], semowner=b_tbB)
                        tr.dma('sp', tbC[:], ropeC[p0:p0 + 128], writes=[b_tbC], semowner=b_tbC)
                    if lat or need_ctx:
                        vn3 = NB[:, 0:A_W].rearrange("p (h w) -> p h w", w=128)
                        norm_heads(pt[:, A_W:2 * A_W].rearrange("p (h w) -> p h w", w=128), bpt, A_H, 128,
                                   avg_b[0][:, :].rearrange("p (h w) -> p h w", w=128), avg_b[1], vn3, b_NB)
                        for c0, cw in pieces(A_W):
                            pp, bp = MMP[lin.i % 2]; lin.i += 1
                            tr.op('pe', lambda e, pp=pp, c0=c0, cw=cw: e.matmul(pp[:, 0:cw], wsT[:], NB[:, c0:c0 + cw], start=True, stop=True), reads=[b_wsT, b_NB], writes=[bp])
                            tr.op('dve', lambda e, pp=pp, c0=c0, cw=cw: e.scalar_tensor_tensor(out=ot[:, c0:c0 + cw], in0=pp[:, 0:cw], scalar=bs_c[:, 0:1], in1=pt[:, c0:c0 + cw], op0=ALU.add, op1=ALU.mult),
                                  reads=[bp, b_bs, bpt, b_ot], writes=[b_ot])
                        tr.dma(Q(), o_d[tok, 0:A_W], ot[:], reads=[b_ot], semowner=b_ot)
                    rms_rows(pt[:, oB:oB + 1536], bpt, 1536, qg_b, cqb[:], b_cqb)
                    transposes_bf16(tps, cqb, b_cqb, 12, cqT, b_cqT)
                    lin.run(cqT, b_cqT, 12, wuq_b, QW, lambda c0, cw, pp, bp: evac_copy(qf[:, c0:c0 + cw], b_qf, pp[:, 0:cw], bp))
                    q3 = qf[:, :].rearrange("p (h w) -> p h w", w=192); x3 = S2[:, 0:B_H * 192].rearrange("p (h w) -> p h w", w=192)
                    def finish_B(dstT):
                        if lat:
                            rope3(x3[:, :, 128:192], b_S2, B_H, 64, tbB, b_tbB)
                        tr.op('act', lambda e: e.copy(out=NB[:, 0:B_H * 192], in_=S2[:, 0:B_H * 192]), reads=[b_S2, b_NB], writes=[b_NB])
                        to_T_store(B_H, 192, dstT, tok)
                    norm_heads(q3, b_qf, B_H, 192, bqn_b[0][:, :].unsqueeze(1).broadcast_to([128, B_H, 192]), bqn_b[1], x3, b_S2)
                    finish_B(qBT)
                    rms_rows(pt[:, oB + 1536:oB + 2048], bpt, 512, kvg_b, cqb[:, 0:512], b_cqb)
                    transposes_bf16(tps, cqb, b_cqb, 4, cqT, b_cqT)
                    lin.run(cqT, b_cqT, 4, wukv_b, KVW, lambda c0, cw, pp, bp: evac_copy(kvf[:, c0:c0 + cw], b_kvf, pp[:, 0:cw], bp))
                    kv3 = kvf[:, :].rearrange("p (h w) -> p h w", w=256)
                    tr.op('pool', lambda e: e.tensor_copy(out=q3[:, :, 0:128], in_=kv3[:, :, 0:128]), reads=[b_kvf, b_qf], writes=[b_qf])
                    tr.op('pool', lambda e: e.tensor_copy(out=q3[:, :, 128:192], in_=pt[:, oB + 2048:oB + 2112].unsqueeze(1).broadcast_to([128, B_H, 64])), reads=[bpt, b_qf], writes=[b_qf])
                    norm_heads(q3, b_qf, B_H, 192, bkn_b[0][:, :].unsqueeze(1).broadcast_to([128, B_H, 192]), bkn_b[1], x3, b_S2)
                    finish_B(kBT)
                    tr.op('act', lambda e: e.copy(out=vb[:, :].rearrange("p (h w) -> p h w", w=128), in_=kv3[:, :, 128:256]), reads=[b_kvf, b_vb], writes=[b_vb])
                    tr.dma(Q(), vB[tok, :], vb[:, :], reads=[b_vb], semowner=b_vb)
                    for (c_off, H, gb, dstT) in ((oC, C_H, cqn_b, qCT), (oC + C_H * 128, C_KV, ckn_b, kCT)):
                        xc3 = S2[:, 0:H * 128].rearrange("p (h w) -> p h w", w=128)
                        norm_heads(pt[:, c_off:c_off + H * 128].rearrange("p (h w) -> p h w", w=128), bpt, H, 128,
                                   gb[0][:, :].unsqueeze(1).broadcast_to([128, H, 128]), gb[1], xc3, b_S2)
                        if lat:
                            rope3(xc3, b_S2, H, 128, tbC, b_tbC)
                        tr.op('act', lambda e, H=H: e.copy(out=NB[:, 0:H * 128], in_=S2[:, 0:H * 128]), reads=[b_S2, b_NB], writes=[b_NB])
                        to_T_store(H, 128, dstT, tok)
                    vc0 = oC + (C_H + C_KV) * 128
                    tr.op('act', lambda e: e.copy(out=vb[:, 0:C_KV * 128], in_=pt[:, vc0:vc0 + C_KV * 128]), reads=[bpt, b_vb], writes=[b_vb])
                    tr.dma(Q(), vC[tok, :], vb[:, 0:C_KV * 128], reads=[b_vb], semowner=b_vb)

        def attention(l, need_ctx):
            with phase() as st:
                kTn, b_kTn = sbt(st, f"kTn{l}", [128, NTOK], BF16); kTr, b_kTr = sbt(st, f"kTr{l}", [64, NTOK], BF16)
                V, b_V = sbt(st, f"V{l}", [128, NT, 129], BF16)
                tr.op('pool', lambda e: e.memset(V[:, :, 128:129], 1.0), writes=[b_V])
                qTn, b_qTn = sbt(st, f"qTn{l}", [128, 512], BF16); qTr, b_qTr = sbt(st, f"qTr{l}", [64, 512], BF16)
                PTr = Ring(tr, st, nc, f"PT{l}", 2, [128, 640], BF16)
                ob, b_ob = sbt(st, f"ob{l}", [128, 4, 128], BF16)
                rd, b_rd = sbt(st, f"rd{l}", [128, 4])
                es, b_es = bload(st, f"es{l}", csink[l], C_H)
                tr.op('act', lambda e: e.activation(out=es[:], in_=es[:], func=AF.Exp), reads=[b_es], writes=[b_es])
                ACC = [BK[3]] + [pst(st, f"acc{l}_{i}", [128, 512]) for i in range(3)]
                SP2 = pst(st, f"sp2{l}", [128, 512])

                def finish(acc_list, ntile, extra_den, col0, tok0):
                    for i in range(ntile):
                        ac, bac = acc_list[i]
                        if extra_den is None:
                            tr.op('dve', lambda e, ac=ac, i=i: e.reciprocal(out=rd[:, i:i + 1], in_=ac[:, 128:129]), reads=[bac, b_rd], writes=[b_rd])
                        else:
                            tr.op('dve', lambda e, ac=ac, i=i: e.tensor_tensor(out=rd[:, i:i + 1], in0=ac[:, 128:129], in1=extra_den, op=ALU.add), reads=[bac, b_es, b_rd], writes=[b_rd])
                            tr.op('dve', lambda e, i=i: e.reciprocal(out=rd[:, i:i + 1], in_=rd[:, i:i + 1]), reads=[b_rd], writes=[b_rd])
                        tr.op('dve', lambda e, ac=ac, i=i: e.tensor_scalar(out=ob[:, i, :], in0=ac[:, 0:128], scalar1=rd[:, i:i + 1], scalar2=None, op0=ALU.mult), reads=[bac, b_rd, b_ob], writes=[b_ob])
                    tr.dma(Q(), o_d[tok0:tok0 + ntile * 128, col0:col0 + 128].rearrange("(t p) d -> p t d", p=128), ob[:, 0:ntile, :], reads=[b_ob], semowner=b_ob)

                sc_b = 192.0 ** -0.5
                for h in range(B_H):
                    tr.dma(Q(), kTn[:], kBT[h, 0:128, :], writes=[b_kTn], semowner=b_kTn)
                    tr.dma(Q(), kTr[:], kBT[h, 128:192, :], writes=[b_kTr], semowner=b_kTr)
                    tr.dma(Q(), V[:, :, 0:128], vB[:, h * 128:(h + 1) * 128].rearrange("(t p) d -> p t d", p=128), writes=[b_V], semowner=b_V)
                    blocks = [(CTX + q0, min(512, SEQ - q0), list(range(NT))) for q0 in range(0, SEQ, 512)]
                    if need_ctx:
                        blocks.append((0, CTX, list(range(NCT))))
                    for (tok0, nq, ktiles) in blocks:
                        nqt = nq // 128
                        tr.dma(Q(), qTn[:, 0:nq], qBT[h, 0:128, tok0:tok0 + nq], writes=[b_qTn], semowner=b_qTn)
                        tr.dma(Q(), qTr[:, 0:nq], qBT[h, 128:192, tok0:tok0 + nq], writes=[b_qTr], semowner=b_qTr)
                        for ki, kt in enumerate(ktiles):
                            pp, bp = MMP[ki % 2]
                            def mm(e, pp=pp, kt=kt, nq=nq):
                                e.matmul(pp[:, 0:nq], kTn[:, kt * 128:(kt + 1) * 128], qTn[:, 0:nq], start=True, stop=False)
                                return e.matmul(pp[:, 0:nq], kTr[:, kt * 128:(kt + 1) * 128], qTr[:, 0:nq], start=False, stop=True)
                            tr.op('pe', mm, reads=[b_kTn, b_kTr, b_qTn, b_qTr], writes=[bp])
                            PT, b_PT = PTr.next()
                            tr.op('act', lambda e, PT=PT, pp=pp, nq=nq: e.activation(out=PT[:, 0:nq], in_=pp[:, 0:nq], func=AF.Exp, scale=sc_b), reads=[bp], writes=[b_PT])
                            for qi in range(nqt):
                                ac, bac = ACC[qi]
                                tr.op('pe', lambda e, ac=ac, PT=PT, qi=qi, kt=kt, ki=ki, n=len(ktiles): e.matmul(ac[:, 0:129], PT[:, qi * 128:(qi + 1) * 128], V[:, kt, :], start=(ki == 0), stop=(ki == n - 1)),
                                      reads=[b_PT, b_V], writes=[bac])
                        finish(ACC, nqt, None, A_W + h * 128, tok0)

                sc_c = 128.0 ** -0.5
                for g in range(C_KV):
                    tr.dma(Q(), kTn[:], kCT[g, :, :], writes=[b_kTn], semowner=b_kTn)
                    tr.dma(Q(), V[:, :, 0:128], vC[:, g * 128:(g + 1) * 128].rearrange("(t p) d -> p t d", p=128), writes=[b_V], semowner=b_V)
                    for hh in range(3):
                        h = g * 3 + hh
                        col0 = A_W + B_H * 128 + h * 128
                        qtiles = list(range(NCT, NT)) + (list(range(NCT)) if need_ctx else [])
                        for b0 in range(0, len(qtiles), 4):
                            grp = qtiles[b0:b0 + 4]
                            tok0 = grp[0] * 128
                            tr.dma(Q(), qTn[:, 0:len(grp) * 128], qCT[h, :, tok0:tok0 + len(grp) * 128], writes=[b_qTn], semowner=b_qTn)
                            for gi, t in enumerate(grp):
                                if t >= NCT:
                                    keys = [(kt, None) for kt in range(NCT)]
                                    if t - 1 >= NCT:
                                        keys.append((t - 1, 0))
                                    keys.append((t, None))
                                    if t + 1 < NT:
                                        keys.append((t + 1, 1))
                                else:
                                    keys = [(kt, None) for kt in range(NCT)]
                                nk = len(keys)
                                pp, bp = (MMP[0] if gi % 2 == 0 else MMP[1])
                                p2, bp2 = SP2
                                def mm(e, pp=pp, p2=p2, keys=keys, gi=gi):
                                    for j, (kt, _) in enumerate(keys):
                                        dst = pp[:, j * 128:(j + 1) * 128] if j < 4 else p2[:, 0:128]
                                        ins = e.matmul(dst, kTn[:, kt * 128:(kt + 1) * 128], qTn[:, gi * 128:(gi + 1) * 128], start=True, stop=True)
                                    return ins
                                tr.op('pe', mm, reads=[b_kTn, b_qTn], writes=[bp] + ([bp2] if nk > 4 else []))
                                PT, b_PT = PTr.next()
                                n1 = min(nk, 4)
                                tr.op('act', lambda e, PT=PT, pp=pp, n1=n1: e.activation(out=PT[:, 0:n1 * 128], in_=pp[:, 0:n1 * 128], func=AF.Exp, scale=sc_c), reads=[bp], writes=[b_PT])
                                if nk > 4:
                                    tr.op('act', lambda e, PT=PT, p2=p2: e.activation(out=PT[:, 512:640], in_=p2[:, 0:128], func=AF.Exp, scale=sc_c), reads=[bp2, b_PT], writes=[b_PT])
                                for j, (kt, m) in enumerate(keys):
                                    if m is not None:
                                        tr.op('dve', lambda e, PT=PT, j=j, m=m: e.tensor_tensor(out=PT[:, j * 128:(j + 1) * 128], in0=PT[:, j * 128:(j + 1) * 128], in1=mk[:, m, :], op=ALU.mult),
                                              reads=[b_PT, b_mk], writes=[b_PT])
                                ac, bac = ACC[gi]
                                def pv(e, ac=ac, PT=PT, keys=keys):
                                    for j, (kt, _) in enumerate(keys):
                                        ins = e.matmul(ac[:, 0:129], PT[:, j * 128:(j + 1) * 128], V[:, kt, :], start=(j == 0), stop=(j == len(keys) - 1))
                                    return ins
                                tr.op('pe', pv, reads=[b_PT, b_V], writes=[bac])
                            finish(ACC, len(grp), es[:, h:h + 1], col0, tok0)

        def out_proj(l, need_ctx):
            with phase() as st:
                gate = [bload(st, f"g2_{l}_{r}", mod_d[r, 2 * D:3 * D], D) for r in range(2 if need_ctx else 1)]
                lin = Lin(st, DC, f"lo{l}", 512, depth=3)
                NP = 2
                otl = [sbt(st, f"otl{l}_{j}", [128, D], BF16) for j in range(NP)]; oT = [sbt(st, f"oT{l}_{j}", [128, DC, 128], BF16) for j in range(NP)]
                xts = [sbt(st, f"xo{l}_{j}", [128, D]) for j in range(NP)]; tmps = [sbt(st, f"to{l}_{j}", [128, 512]) for j in range(NP)]
                tps = [pst(st, f"tpo{l}_{i}", [128, 4, 128], BF16) for i in range(2)]
                tl = (list(range(NT)) if need_ctx else list(range(NCT, NT)))
                for t0 in range(0, len(tl), NP):
                    acts = []
                    for j, t in enumerate(tl[t0:t0 + NP]):
                        r = 1 if t < NCT else 0
                        tok = slice(t * 128, (t + 1) * 128)
                        tr.dma(Q(), otl[j][0][:], o_d[tok, :], writes=[otl[j][1]], semowner=otl[j][1])
                        tr.dma(Q(), xts[j][0][:], xres[tok, :], writes=[xts[j][1]], semowner=xts[j][1])
                        transposes_bf16(tps, otl[j][0], otl[j][1], DC, oT[j][0], oT[j][1])
                        def ev(c0, cw, pp, bp, r=r, j=j):
                            tmp, b_tmp = tmps[j]; xt, bxt = xts[j]
                            tr.op('dve', lambda e: e.tensor_tensor(out=tmp[:, 0:cw], in0=pp[:, 0:cw], in1=gate[r][0][:, c0:c0 + cw], op=ALU.mult), reads=[bp, gate[r][1], b_tmp], writes=[b_tmp])
                            tr.op('pool', lambda e: e.tensor_tensor(out=xt[:, c0:c0 + cw], in0=xt[:, c0:c0 + cw], in1=tmp[:, 0:cw], op=ALU.add), reads=[b_tmp, bxt], writes=[bxt])
                        acts.append((oT[j][0], oT[j][1], ev))
                    lin_multi(lin, acts, DC, wout_b, D)
                    for j, t in enumerate(tl[t0:t0 + NP]):
                        tr.dma(Q(), xres[t * 128:(t + 1) * 128, :], xts[j][0][:], reads=[xts[j][1]], semowner=xts[j][1])

        def peer(l, need_ctx):
            tiles = list(range(NT)) if need_ctx else list(range(NCT, NT))
            NB3 = 3
            with phase() as st:
                skT, b_skT = sbt(st, f"skT{l}", [128, 16, 128], BF16)
                for hp in range(16):
                    t_o, b_o = transpose_rows(st, psk[l, hp], 128, f"skf{l}_{hp}")
                    tr.op('dve', lambda e, hp=hp, t_o=t_o: e.tensor_copy(out=skT[:, hp, :], in_=t_o[:]), reads=[b_o, b_skT], writes=[b_skT])
                gT, b_gT = sbt(st, f"gT{l}", [128, DC, NB3 * 128], BF16)
                sc, b_sc = sbt(st, f"sc{l}", [128, NB3, 16, 128])
                TAU, b_TAU = sbt(st, f"tau{l}", [128, NB3, 8]); BI2, b_BI2 = sbt(st, f"bi2{l}", [128, NB3, 8])
                EB, b_EB = sbt(st, f"eb{l}", [128, NB3, 8])
                acc, b_acc = sbt(st, f"acc{l}", [128, NB3, D])
                for b0 in range(0, len(tiles), NB3):
                    blk = tiles[b0:b0 + NB3]
                    nb = len(blk); ntok = nb * 128
                    with phase() as s2:
                        LT = ln_temps(s2, f"lp{l}_{b0}")
                        lin = Lin(s2, DC, f"lq{l}_{b0}", 512 if DC <= 16 else 256, depth=3)
                        qtoks = [sbt(s2, f"qtok{l}_{b0}_{j}", [128, 2048], BF16) for j in range(NB3)]
                        qT, b_qT = sbt(s2, f"qT{l}_{b0}", [128, 16, 128], BF16)
                        T16, b_T16 = sbt(s2, f"T16{l}_{b0}", [128, 16, 16]); tm, b_tm = sbt(s2, f"tm{l}_{b0}", [128, 256]); cd, b_cd = sbt(s2, f"cd{l}_{b0}", [128, 256])
                        B16, b_B16 = sbt(s2, f"B16{l}_{b0}", [128, 8, 16]); MX, b_MX = sbt(s2, f"MX{l}_{b0}", [128, 8]); ZS, b_ZS = sbt(s2, f"ZS{l}_{b0}", [128, 8])
                        jk, b_jk = sbt(s2, f"jkz{l}_{b0}", [128, 16])
                        acts = []
                        for i, t in enumerate(blk):
                            gv = gT[:, :, i * 128:(i + 1) * 128]
                            ln_mod_T(LT, t, K2['A2'], K2['bA2'], K2['B2'], K2['bB2'], gv, b_gT)
                            acts.append((gv, b_gT, (lambda c0, cw, pp, bp, i=i: evac_copy(qtoks[i][0][:, c0:c0 + cw], qtoks[i][1], pp[:, 0:cw], bp))))
                        lin_multi(lin, acts, DC, wq_b, 2048)
                        for i, t in enumerate(blk):
                            qtok, b_qtok = qtoks[i]
                            transposes_bf16(LT['pr'], qtok, b_qtok, 16, qT, b_qT)
                            for h4 in range(0, 16, 4):
                                pp, bp = MMP[(h4 // 4) % 2]
                                def mm(e, pp=pp, h4=h4):
                                    for j in range(4):
                                        ins = e.matmul(pp[:, j * 128:(j + 1) * 128], qT[:, h4 + j, :], skT[:, h4 + j, :], start=True, stop=True)
                                    return ins
                                tr.op('pe', mm, reads=[b_qT, b_skT], writes=[bp])
                                evac_copy(sc[:, i, h4:h4 + 4, :], b_sc, pp[:, :].rearrange("p (a b) -> p a b", b=128), bp)
                            for hp in range(16):
                                tr.op('dve', lambda e, hp=hp: e.max(out=T16[:, hp, 0:8], in_=sc[:, i, hp, :]), reads=[b_sc, b_T16], writes=[b_T16])
                                tr.op('dve', lambda e, hp=hp: e.match_replace(out=tm[:, 0:128], in_to_replace=T16[:, hp, 0:8], in_values=sc[:, i, hp, :], imm_value=-1e30), reads=[b_sc, b_T16], writes=[b_tm])
                                tr.op('dve', lambda e, hp=hp: e.max(out=T16[:, hp, 8:16], in_=tm[:, 0:128]), reads=[b_tm, b_T16], writes=[b_T16])
                            for h in range(8):
                                cd3 = cd[:, :].rearrange("p (a b) -> p a b", b=16)
                                tr.op('dve', lambda e, h=h: e.tensor_tensor(out=cd3, in0=T16[:, 2 * h, :].unsqueeze(2).broadcast_to([128, 16, 16]),
                                                                       in1=T16[:, 2 * h + 1, :].unsqueeze(1).broadcast_to([128, 16, 16]), op=ALU.add), reads=[b_T16], writes=[b_cd])
                                tr.op('dve', lambda e, h=h: e.max(out=B16[:, h, 0:8], in_=cd[:, :]), reads=[b_cd, b_B16], writes=[b_B16])
                                tr.op('dve', lambda e, h=h: e.match_replace(out=tm[:, :], in_to_replace=B16[:, h, 0:8], in_values=cd[:, :], imm_value=-1e30), reads=[b_cd, b_B16], writes=[b_tm])
                                tr.op('dve', lambda e, h=h: e.max(out=B16[:, h, 8:16], in_=tm[:, :]), reads=[b_tm, b_B16], writes=[b_B16])
                            tr.op('dve', lambda e: e.tensor_reduce(out=TAU[:, i, :], in_=B16[:, :, 8:16], axis=AX.X, op=ALU.min), reads=[b_B16, b_TAU], writes=[b_TAU])
                            tr.op('dve', lambda e: e.tensor_reduce(out=MX[:, :], in_=B16[:, :, 0:8], axis=AX.X, op=ALU.max), reads=[b_B16], writes=[b_MX])
                            tr.op('dve', lambda e: e.tensor_scalar(out=MX[:, :], in0=MX[:, :], scalar1=-1.0, scalar2=None, op0=ALU.mult), reads=[b_MX], writes=[b_MX])
                            for h in range(8):
                                tr.op('act', lambda e, h=h: e.activation(out=jk[:, :], in_=B16[:, h, :], func=AF.Exp, bias=MX[:, h:h + 1], accum_out=ZS[:, h:h + 1]),
                                      reads=[b_B16, b_MX, b_jk, b_ZS], writes=[b_jk, b_ZS])
                            tr.op('act', lambda e: e.activation(out=ZS[:, :], in_=ZS[:, :], func=AF.Ln), reads=[b_ZS], writes=[b_ZS])
                            tr.op('dve', lambda e: e.tensor_tensor(out=BI2[:, i, :], in0=TAU[:, i, :], in1=MX[:, :], op=ALU.add), reads=[b_TAU, b_MX, b_BI2], writes=[b_BI2])
                            tr.op('dve', lambda e: e.tensor_tensor(out=BI2[:, i, :], in0=BI2[:, i, :], in1=ZS[:, :], op=ALU.subtract), reads=[b_ZS, b_BI2], writes=[b_BI2])
                            tr.op('act', lambda e: e.activation(out=EB[:, i, :], in_=BI2[:, i, :], func=AF.Exp), reads=[b_BI2, b_EB], writes=[b_EB])
                    with phase() as s2:
                        putr = Ring(tr, s2, nc, f"put{l}_{b0}", 3, [128, DC, 128], BF16)
                        pvr = Ring(tr, s2, nc, f"pv{l}_{b0}", 2, [128, 16, 256], BF16)
                        WTr = Ring(tr, s2, nc, f"WT{l}_{b0}", 2, [128, 16, NB3 * 128], BF16)
                        aTr = Ring(tr, s2, nc, f"aT{l}_{b0}", 2, [128, NB3 * 128], BF16)
                        a8r = Ring(tr, s2, nc, f"a8{l}_{b0}", 2, [128, NB3, 8], F32)
                        ddr = Ring(tr, s2, nc, f"dd{l}_{b0}", 2, [128, NB3, 8, 128], BF16)
                        eer = Ring(tr, s2, nc, f"ee{l}_{b0}", 2, [128, NB3, 8, 128], BF16)
                        DG, b_DG = sbt(s2, f"DG{l}_{b0}", [128, NB3, 8, 128], BF16)
                        for i in range(nb):
                            for h in range(8):
                                tr.op('pool', lambda e, i=i, h=h: e.tensor_scalar(out=DG[:, i, h, :], in0=idb[:], scalar1=EB[:, i, h:h + 1], scalar2=None, op0=ALU.mult),
                                      reads=[b_idb, b_EB, b_DG], writes=[b_DG])
                        GPS = [BK[3], pst(s2, f"gps{l}_{b0}", [128, 512])]
                        VPS = [pst(s2, f"vps{l}_{b0}_{i}", [128, 512]) for i in range(2)]
                        vi = 0
                        for eb in range(8):
                            WT, b_WT = WTr.next()
                            for ec in range(16):
                                c = eb * 16 + ec
                                pu_t, b_pu = putr.next()
                                tr.dma(Q(), pu_t, put_b[c].rearrange("p (k e) -> p k e", e=128), writes=[b_pu], semowner=b_pu)
                                pp, bp = MMP[c % 2]
                                def mm(e, pp=pp, pu_t=pu_t):
                                    for k in range(DC):
                                        ins = e.matmul(pp[:, 0:ntok], pu_t[:, k, :], gT[:, k, 0:ntok], start=(k == 0), stop=(k == DC - 1))
                                    return ins
                                tr.op('pe', mm, reads=[b_pu, b_gT], writes=[bp])
                                aT, b_aT = aTr.next()
                                tr.op('act', lambda e, pp=pp, aT=aT: e.activation(out=aT[:, 0:ntok], in_=pp[:, 0:ntok], func=AF.Gelu), reads=[bp], writes=[b_aT])
                                gp, bgp = GPS[c % 2]
                                scv = sc[:, 0:nb, :, :].rearrange("p i (h two) n -> p i h two n", two=2)
                                s1c = scv[:, :, :, 0, c]
                                s2v = scv[:, :, :, 1, :]
                                a8, b_a8 = a8r.next(); dd, b_dd = ddr.next(); ee, b_ee = eer.next(); Gh, b_Gh = ee, b_ee
                                tr.op('dve', lambda e, s1c=s1c, a8=a8: e.tensor_tensor(out=a8[:, 0:nb, :], in0=s1c, in1=TAU[:, 0:nb, :], op=ALU.subtract), reads=[b_sc, b_TAU], writes=[b_a8])
                                tr.op('dve', lambda e, s2v=s2v, a8=a8, dd=dd: e.tensor_tensor(out=dd[:, 0:nb], in0=s2v, in1=a8[:, 0:nb, :].unsqueeze(3).broadcast_to([128, nb, 8, 128]), op=ALU.add),
                                      reads=[b_sc, b_a8], writes=[b_dd])
                                tr.op('act', lambda e, dd=dd, ee=ee: e.activation(out=ee[:, 0:nb], in_=dd[:, 0:nb], func=AF.Exp), reads=[b_dd], writes=[b_ee])
                                tr.op('dve', lambda e, dd=dd, ee=ee, Gh=Gh: e.scalar_tensor_tensor(out=Gh[:, 0:nb], in0=dd[:, 0:nb], scalar=0.0, in1=ee[:, 0:nb], op0=ALU.is_ge, op1=ALU.mult),
                                      reads=[b_dd, b_ee], writes=[b_Gh])
                                def gm(e, gp=gp, Gh=Gh):
                                    for i in range(nb):
                                        for h in range(8):
                                            ins = e.matmul(gp[:, i * 128:(i + 1) * 128], Gh[:, i, h, :], DG[:, i, h, :], start=(h == 0), stop=(h == 7))
                                    return ins
                                tr.op('pe', gm, reads=[b_Gh, b_DG], writes=[bgp])
                                tr.op('dve', lambda e, gp=gp, WT=WT, ec=ec, aT=aT: e.tensor_tensor(out=WT[:, ec, 0:ntok], in0=gp[:, 0:ntok], in1=aT[:, 0:ntok], op=ALU.mult), reads=[bgp, b_aT, b_WT], writes=[b_WT])
                            for c0, cw in pieces(D, 256):
                                pv_t, b_pv = pvr.next()
                                tr.dma(Q(), pv_t[:, :, 0:cw], pv_b[eb * 2048:(eb + 1) * 2048, c0:c0 + cw].rearrange("(c p) d -> p c d", p=128), writes=[b_pv], semowner=b_pv)
                                for i in range(nb):
                                    vp, bvp = VPS[vi % 2]; vi += 1
                                    def vm(e, vp=vp, pv_t=pv_t, i=i, cw=cw, WT=WT):
                                        for ec in range(16):
                                            ins = e.matmul(vp[:, 0:cw], WT[:, ec, i * 128:(i + 1) * 128], pv_t[:, ec, 0:cw], start=(ec == 0), stop=(ec == 15))
                                        return ins
                                    tr.op('pe', vm, reads=[b_WT, b_pv], writes=[bvp])
                                    if eb == 0:
                                        evac_copy(acc[:, i, c0:c0 + cw], b_acc, vp[:, 0:cw], bvp)
                                    else:
                                        tr.op('dve', lambda e, vp=vp, i=i, c0=c0, cw=cw: e.tensor_tensor(out=acc[:, i, c0:c0 + cw], in0=acc[:, i, c0:c0 + cw], in1=vp[:, 0:cw], op=ALU.add), reads=[bvp, b_acc], writes=[b_acc])
                    with phase() as s2:
                        gate = [bload(s2, f"g5_{l}_{b0}_{r}", mod_d[r, 5 * D:6 * D], D) for r in range(2 if need_ctx else 1)]
                        xr = Ring(tr, s2, nc, f"xp{l}_{b0}", 2, [128, D], F32)
                        for i, t in enumerate(blk):
                            r = 1 if t < NCT else 0
                            xt, bxt = xr.next()
                            tok = slice(t * 128, (t + 1) * 128)
                            tr.dma(Q(), xt, xres[tok, :], writes=[bxt], semowner=bxt)
                            tr.op('dve', lambda e, i=i, r=r: e.tensor_tensor(out=acc[:, i, :], in0=acc[:, i, :], in1=gate[r][0][:, :], op=ALU.mult), reads=[gate[r][1], b_acc], writes=[b_acc])
                            tr.op('pool', lambda e, i=i, xt=xt: e.tensor_tensor(out=xt, in0=xt, in1=acc[:, i, :], op=ALU.add), reads=[b_acc, bxt], writes=[bxt])
                            tr.dma(Q(), xres[tok, :], xt, reads=[bxt], semowner=bxt)

        for l in range(L):
            need_ctx = l < L - 1
            with phase() as st:
                cast_copy(st, w_in[l], win_b, D, P_IN, B_w['win'], f"cw{l}")
            with phase() as st:
                cast_copy(st, wuq[l], wuq_b, 1536, QW, B_w['wuq'], f"cwa{l}")
                cast_copy(st, wukv[l], wukv_b, 512, KVW, B_w['wukv'], f"cwb{l}")
            with phase() as st:
                cast_copy(st, w_out[l], wout_b, D, D, B_w['wout'], f"cwc{l}")
                cast_copy(st, pwq[l], wq_b, D, 2048, B_w['wq'], f"cwd{l}")
            stop = debug[1] if (debug and debug[0] == 'stop') else None
            order = ['tables', 'mod', 'mixer_prep', 'attention', 'out_proj', 'peer']
            upto = order.index(stop) if stop else len(order) - 1
            if (debug and debug[0] == 'dram' and debug[2:] and debug[2] == ('stop_mid', l)):
                upto = order.index('out_proj')
            if upto >= 0 and not (stop and stop.startswith('no_tables')):
                prep_tables(l)
            if upto >= 1:
                phase_mod(l)
            if upto >= 2:
                with phase() as st:
                    K2 = load_layer_consts(st, l)
                    mixer_prep(l, K2, need_ctx)
                    if upto >= 3:
                        attention(l, need_ctx)
                    if upto >= 4:
                        out_proj(l, need_ctx)
                    if upto >= 5:
                        peer(l, need_ctx)

        if dumps:
            with phase() as st:
                for di, (v2, od) in enumerate(dumps):
                    ring = Ring(tr, st, nc, f"dmp{di}", 2, [128, v2.shape[1]], v2.dtype)
                    for r0 in range(0, v2.shape[0], 128):
                        r1 = min(r0 + 128, v2.shape[0])
                        ap, b = ring.next()
                        tr.dma('sp', ap[0:r1 - r0, :], v2[r0:r1, :], writes=[b], semowner=b)
                        tr.dma('sp', od[r0:r1, :], ap[0:r1 - r0, :], reads=[b], semowner=b)
        with phase() as st:
            ring = Ring(tr, st, nc, "ocp", 3, [128, D], F32)
            toks = []
            for t in range(NCT, NT):
                ap, b = ring.next()
                q = Q()
                tr.dma(q, ap, xres[t * 128:(t + 1) * 128, :], reads=[B_xres[t]], writes=[b], semowner=b)
                toks.append((q, tr.dma(q, out[(t - NCT) * 128:(t - NCT + 1) * 128, :], ap, reads=[b], writes=[B_out], semowner=b)))
            for q, tk in toks[-6:]:
                tr.wait_all(q, [tk])
                tr.wait_all('sp', [tk])
    return nc


def rope_tables(SEQ, rot, grid_w=64):
    rows = SEQ // grid_w
    row = np.repeat(np.arange(rows, dtype=np.float32), grid_w)
    col = np.tile(np.arange(grid_w, dtype=np.float32), rows)
    nf = rot // 4
    inv = (10000.0 ** (-np.arange(nf, dtype=np.float32) / nf)).astype(np.float32)
    ar = row[:, None] * inv
    ac = col[:, None] * inv
    ang = np.concatenate([ar, ar, ac, ac], axis=-1)
    cos, sin = np.cos(ang).astype(np.float32), np.sin(ang).astype(np.float32)
    sgn = np.tile(np.concatenate([-np.ones(nf), np.ones(nf)]), 2).astype(np.float32)
    return np.stack([cos, sin * sgn], axis=1)


def make_in_maps(cfg, inputs):
    D, SEQ, CTX, L, DC = cfg['D'], cfg['SEQ'], cfg['CTX'], cfg['DEPTH'], cfg['DC']
    f = lambda a: np.ascontiguousarray(np.asarray(a, dtype=np.float32))
    shared = {k: f(inputs[k]) for k in ('w_mod', 'b_mod', 'w_in', 'a_v_gain', 'a_w_s', 'a_b_s', 'b_q_gain', 'b_kv_gain',
                                        'b_w_uq', 'b_w_ukv', 'b_qn_gain', 'b_kn_gain', 'c_qn_gain', 'c_kn_gain', 'c_sink',
                                        'w_out', 'peer_w_q', 'peer_u', 'peer_v')}
    shared['norm1_gain'] = f(inputs['norm1_gain']).reshape(L, DC, 128)
    shared['norm2_gain'] = f(inputs['norm2_gain']).reshape(L, DC, 128)
    shared['peer_subkeys'] = f(inputs['peer_subkeys']).reshape(L, 16, 128, 128)
    shared['c_ctx'] = f(inputs['c_ctx']).reshape(DC, 128)
    shared['ropeB'] = rope_tables(SEQ, 64)
    shared['ropeC'] = rope_tables(SEQ, 128)
    j = np.arange(128)[:, None]; i = np.arange(128)[None, :]
    shared['masks'] = np.stack([(j >= i), (j <= i)]).astype(np.float32)
    maps = []
    for b in range(cfg['BATCH']):
        m = dict(shared)
        m['x'] = f(inputs['x'][b]); m['ctx'] = f(inputs['ctx'][b]); m['c'] = f(inputs['c'][b]).reshape(DC, 128)
        maps.append(m)
    return maps


def kernel(**inputs):
    cfg = make_cfg()
    nc = build(cfg)
    maps = make_in_maps(cfg, inputs)
    res = run_bass_kernel_spmd(nc, maps, core_ids=list(range(cfg['BATCH'])))
    return np.stack([r['out'] for r in res.results], axis=0).astype(np.float32)
```
